# Optimizing a Trainium2 kernel written in Bass

```python
import math
import jax, jax.numpy as jnp
from jax import lax
import numpy as np

D_MODEL = 1024
BATCH = 4
SEQ = 8192
DEPTH = 2

GRID_W = 64
CTX_LEN = 256
A_HEADS = 4
A_HEAD_DIM = 64
A_WIDTH = A_HEADS * 2 * A_HEAD_DIM
B_HEADS = 8
B_HEAD_DIM = 64
B_WIDTH = B_HEADS * B_HEAD_DIM
NA_ROWS = 8
NA_COLS = 16
C_HEADS = 8
C_NOPE = 64
C_ROPE = 32
C_VDIM = 64
C_Q_RANK = 384
C_KV_RANK = 256
C_WIDTH = C_HEADS * C_VDIM
N_BRANCH = 3
IN_SIZES = (A_WIDTH, A_WIDTH, A_WIDTH, B_WIDTH, B_WIDTH, B_WIDTH, C_Q_RANK, C_KV_RANK, C_ROPE, N_BRANCH * D_MODEL)
IN_SPLITS = tuple(sum(IN_SIZES[:i + 1]) for i in range(len(IN_SIZES) - 1))
D_IN = sum(IN_SIZES)
N_EXPERTS = 32
TOP_K = 4
D_FF = D_MODEL
SWIGLU_LIMIT = 7.0
SWIGLU_ALPHA = 1.702
N_MOD = 6
ROPE_BASE = 10000.0
EPS = 1e-6
NEG_INF = -1e30
Q_BLOCK = 128
E_BLOCK = 128

kernel_name = 'hybrid_diffusion_trunk_ctx_prefix'


def rmsnorm(x, g):
    xf = x.astype(jnp.float32)
    y = xf * lax.rsqrt(jnp.mean(xf * xf, axis=-1, keepdims=True) + EPS)
    return (y * g.astype(jnp.float32)).astype(x.dtype)


def modulate(x, g, shift, scale):
    return rmsnorm(x, g) * (1.0 + scale) + shift


def axial_tables(n, rot_dim, dtype):
    pos = jnp.arange(n, dtype=jnp.int32)
    rows = (pos // GRID_W).astype(jnp.float32)
    cols = (pos % GRID_W).astype(jnp.float32)
    n_freq = rot_dim // 4
    inv = ROPE_BASE ** (-jnp.arange(n_freq, dtype=jnp.float32) / n_freq)
    ang = jnp.concatenate([rows[:, None] * inv, cols[:, None] * inv], axis=-1)
    return jnp.cos(ang).astype(dtype), jnp.sin(ang).astype(dtype)


def rope_axial(x, cos, sin):
    d = x.shape[-1]
    qd = d // 4
    shape = (x.shape[1],) + (1,) * (x.ndim - 3) + (2 * qd,)
    cos = cos.reshape(shape)
    sin = sin.reshape(shape)

    def rot(h, cs, sn):
        a, b = h[..., :qd], h[..., qd:]
        return jnp.concatenate([a * cs - b * sn, b * cs + a * sn], axis=-1)

    return jnp.concatenate([rot(x[..., :2 * qd], cos[..., :qd], sin[..., :qd]),
                            rot(x[..., 2 * qd:], cos[..., qd:], sin[..., qd:])], axis=-1)


def blocked_attention(q, k, v, scale):
    bsz, nq, nh, dk = q.shape
    qb = min(Q_BLOCK, nq)
    q_blocks = jnp.moveaxis(q.reshape(bsz, nq // qb, qb, nh, dk), 1, 0)

    def one(qblk):
        s = jnp.einsum('bqhd,bkhd->bhqk', qblk, k).astype(jnp.float32) * scale
        p = jax.nn.softmax(s, axis=-1).astype(v.dtype)
        return jnp.einsum('bhqk,bkhe->bqhe', p, v)

    out = lax.map(one, q_blocks)
    return jnp.moveaxis(out, 0, 1).reshape(bsz, nq, nh, v.shape[-1])


def blocked_diff_attention(q, k, v, lam, scale):
    bsz, nq = q.shape[0], q.shape[1]
    qb = min(Q_BLOCK, nq)
    q_blocks = jnp.moveaxis(q.reshape((bsz, nq // qb, qb) + q.shape[2:]), 1, 0)

    def one(qblk):
        s = jnp.einsum('bqhcd,bkhcd->bhcqk', qblk, k).astype(jnp.float32) * scale
        p = jax.nn.softmax(s, axis=-1)
        a = (p[:, :, 0] - lam * p[:, :, 1]).astype(v.dtype)
        return jnp.einsum('bhqk,bkhe->bqhe', a, v)

    out = lax.map(one, q_blocks)
    return jnp.moveaxis(out, 0, 1).reshape((bsz, nq) + v.shape[2:])


def neighbourhood_attention(q, k, v, k_ctx, v_ctx, rpb):
    bsz, n, nh, d = q.shape
    rows = n // GRID_W
    win_r = min(NA_ROWS, rows)
    scale = d ** -0.5
    qg = q.reshape(bsz, rows, GRID_W, nh, d)
    kg = k.reshape(bsz, rows, GRID_W, nh, d)
    vg = v.reshape(bsz, rows, GRID_W, nh, d)
    r_idx = jnp.arange(rows, dtype=jnp.int32)
    row_start = jnp.clip(r_idx - NA_ROWS // 2, 0, rows - win_r)
    c_idx = jnp.arange(GRID_W, dtype=jnp.int32)
    col_start = jnp.clip(c_idx - NA_COLS // 2, 0, GRID_W - NA_COLS)
    col_mask = (c_idx[None, :] >= col_start[:, None]) & (c_idx[None, :] < col_start[:, None] + NA_COLS)
    col_bias_idx = jnp.clip(c_idx[None, :] - c_idx[:, None] + NA_COLS - 1, 0, 2 * NA_COLS - 2)
    rpb_c = rpb[:, :, col_bias_idx]
    n_lat = win_r * GRID_W

    def one(args):
        q_r, r, rs = args
        k_blk = lax.dynamic_slice_in_dim(kg, rs, win_r, axis=1)
        v_blk = lax.dynamic_slice_in_dim(vg, rs, win_r, axis=1)
        bias = jnp.take(rpb_c, rs + jnp.arange(win_r) - r + NA_ROWS - 1, axis=1)
        s = jnp.einsum('bqhd,bikhd->bhqik', q_r, k_blk).astype(jnp.float32) * scale
        s = s + jnp.transpose(bias, (0, 2, 1, 3))[None].astype(jnp.float32)
        s = jnp.where(col_mask[:, None, :], s, NEG_INF)
        s_ctx = jnp.einsum('bqhd,bkhd->bhqk', q_r, k_ctx).astype(jnp.float32) * scale
        s_all = jnp.concatenate([s.reshape(bsz, nh, GRID_W, n_lat), s_ctx], axis=-1)
        p = jax.nn.softmax(s_all, axis=-1).astype(v.dtype)
        p_lat = p[..., :n_lat].reshape(bsz, nh, GRID_W, win_r, GRID_W)
        return (jnp.einsum('bhqik,bikhe->bqhe', p_lat, v_blk)
                + jnp.einsum('bhqk,bkhe->bqhe', p[..., n_lat:], v_ctx))

    out = lax.map(one, (jnp.moveaxis(qg, 1, 0), r_idx, row_start))
    return jnp.moveaxis(out, 0, 1).reshape(bsz, n, nh, d)


def diff_qkv(qa, ka, va, rope):
    q = qa.reshape(qa.shape[:-1] + (A_HEADS, 2, A_HEAD_DIM))
    k = ka.reshape(ka.shape[:-1] + (A_HEADS, 2, A_HEAD_DIM))
    v = va.reshape(va.shape[:-1] + (A_HEADS, 2 * A_HEAD_DIM))
    if rope is not None:
        q = rope_axial(q, *rope)
        k = rope_axial(k, *rope)
    return q, k, v


def diff_post(o, g_subln, lam_init):
    o = rmsnorm(o, g_subln) * (1.0 - lam_init)
    return o.reshape(o.shape[:-2] + (A_WIDTH,))


def heads(t, nh):
    return t.reshape(t.shape[:-1] + (nh, t.shape[-1] // nh))


def mla_qkv(q_lat, kv_lat, k_rope, g_q_a, w_q_b, g_kv_a, w_kv_b, rope):
    q = heads(rmsnorm(q_lat, g_q_a) @ w_q_b, C_HEADS)
    kv = heads(rmsnorm(kv_lat, g_kv_a) @ w_kv_b, C_HEADS)
    q_nope, q_pe = q[..., :C_NOPE], q[..., C_NOPE:]
    k_nope, v = kv[..., :C_NOPE], kv[..., C_NOPE:]
    k_pe = k_rope[:, :, None, :]
    if rope is not None:
        q_pe = rope_axial(q_pe, *rope)
        k_pe = rope_axial(k_pe, *rope)
    k_pe = jnp.broadcast_to(k_pe, k_nope.shape[:-1] + (C_ROPE,))
    return (jnp.concatenate([q_nope, q_pe], axis=-1), jnp.concatenate([k_nope, k_pe], axis=-1), v)


def merge_branches(ya, yb, yc, gates, w_br_a, w_br_b, w_br_c, w_out):
    g = jax.nn.sigmoid(gates.astype(jnp.float32)).astype(ya.dtype)
    g = g.reshape(g.shape[:-1] + (N_BRANCH, D_MODEL))
    m = g[..., 0, :] * (ya @ w_br_a) + g[..., 1, :] * (yb @ w_br_b) + g[..., 2, :] * (yc @ w_br_c)
    return m @ w_out


def moe_ffn(h, w_router, b_router, w_gate_up, b_gate_up, w_down, b_down):
    shp = h.shape
    ht = h.reshape(-1, shp[-1])
    n_tok = ht.shape[0]
    logits = (ht @ w_router).astype(jnp.float32) + b_router.astype(jnp.float32)
    top_val, top_idx = lax.top_k(logits, TOP_K)
    gate_w = jax.nn.softmax(top_val, axis=-1)
    m = n_tok * TOP_K
    flat_e = top_idx.reshape(m)
    flat_t = jnp.repeat(jnp.arange(n_tok, dtype=jnp.int32), TOP_K)
    order = jnp.argsort(flat_e)
    e_s, t_s, w_s = flat_e[order], flat_t[order], gate_w.reshape(m)[order]
    counts = jnp.zeros((N_EXPERTS,), jnp.int32).at[flat_e].add(1)
    start = jnp.cumsum(counts) - counts
    padded = ((counts + E_BLOCK - 1) // E_BLOCK) * E_BLOCK
    pend = jnp.cumsum(padded)
    pstart = pend - padded
    dest = pstart[e_s] + jnp.arange(m, dtype=jnp.int32) - start[e_s]
    n_blk = -(-m // E_BLOCK) + N_EXPERTS
    n_rows = n_blk * E_BLOCK
    buf_t = jnp.full((n_rows,), n_tok, jnp.int32).at[dest].set(t_s)
    buf_w = jnp.zeros((n_rows,), jnp.float32).at[dest].set(w_s)
    blk_e = jnp.clip(jnp.searchsorted(pend, jnp.arange(n_blk, dtype=jnp.int32) * E_BLOCK, side='right'), 0, N_EXPERTS - 1)
    x_pad = jnp.concatenate([ht, jnp.zeros((1, ht.shape[-1]), ht.dtype)], axis=0)

    def expert_block(args):
        tok, e = args
        gu = x_pad[tok] @ w_gate_up[e] + b_gate_up[e]
        gate = jnp.minimum(gu[:, 0::2], SWIGLU_LIMIT)
        up = jnp.clip(gu[:, 1::2], -SWIGLU_LIMIT, SWIGLU_LIMIT)
        act = (up + 1.0) * (gate * jax.nn.sigmoid(SWIGLU_ALPHA * gate))
        return act @ w_down[e] + b_down[e]

    y = lax.map(expert_block, (buf_t.reshape(n_blk, E_BLOCK), blk_e)).reshape(n_rows, -1)
    out = jax.ops.segment_sum(y * buf_w[:, None].astype(y.dtype), buf_t, num_segments=n_tok + 1)[:n_tok]
    return out.reshape(shp)


def setup_inputs(seed: int = 0) -> dict:
    key = jax.random.key(seed)
    ks = jax.random.split(key, 30)
    f32 = jnp.float32

    def nrm(k, shape, scale):
        return jax.random.normal(k, shape, f32) * scale

    def gain(k, shape):
        return 1.0 + 0.05 * jax.random.normal(k, shape, f32)

    L = DEPTH
    return {
        'x': nrm(ks[0], (BATCH, SEQ, D_MODEL), 1.0),
        'c': nrm(ks[1], (BATCH, D_MODEL), 1.0),
        'ctx': nrm(ks[2], (BATCH, CTX_LEN, D_MODEL), 1.0),
        'c_ctx': nrm(ks[3], (D_MODEL,), 1.0),
        'w_mod': nrm(ks[4], (L, D_MODEL, N_MOD * D_MODEL), 0.5 * D_MODEL ** -0.5),
        'b_mod': nrm(ks[5], (L, N_MOD * D_MODEL), 0.01),
        'g_mix': gain(ks[6], (L, D_MODEL)),
        'w_in': nrm(ks[7], (L, D_MODEL, D_IN), D_MODEL ** -0.5),
        'lam_q1': nrm(ks[8], (L, A_HEAD_DIM), 0.1),
        'lam_k1': nrm(ks[9], (L, A_HEAD_DIM), 0.1),
        'lam_q2': nrm(ks[10], (L, A_HEAD_DIM), 0.1),
        'lam_k2': nrm(ks[11], (L, A_HEAD_DIM), 0.1),
        'g_subln': gain(ks[12], (L, 2 * A_HEAD_DIM)),
        'rpb': nrm(ks[13], (L, B_HEADS, 2 * NA_ROWS - 1, 2 * NA_COLS - 1), 0.1),
        'g_q_a': gain(ks[14], (L, C_Q_RANK)),
        'w_q_b': nrm(ks[15], (L, C_Q_RANK, C_HEADS * (C_NOPE + C_ROPE)), C_Q_RANK ** -0.5),
        'g_kv_a': gain(ks[16], (L, C_KV_RANK)),
        'w_kv_b': nrm(ks[17], (L, C_KV_RANK, C_HEADS * (C_NOPE + C_VDIM)), C_KV_RANK ** -0.5),
        'w_br_a': nrm(ks[18], (L, A_WIDTH, D_MODEL), A_WIDTH ** -0.5),
        'w_br_b': nrm(ks[19], (L, B_WIDTH, D_MODEL), B_WIDTH ** -0.5),
        'w_br_c': nrm(ks[20], (L, C_WIDTH, D_MODEL), C_WIDTH ** -0.5),
        'w_out': nrm(ks[21], (L, D_MODEL, D_MODEL), D_MODEL ** -0.5),
        'g_ffn': gain(ks[22], (L, D_MODEL)),
        'w_router': nrm(ks[23], (L, D_MODEL, N_EXPERTS), D_MODEL ** -0.5),
        'b_router': nrm(ks[24], (L, N_EXPERTS), 0.01),
        'w_gate_up': nrm(ks[25], (L, N_EXPERTS, D_MODEL, 2 * D_FF), D_MODEL ** -0.5),
        'b_gate_up': nrm(ks[26], (L, N_EXPERTS, 2 * D_FF), 0.01),
        'w_down': nrm(ks[27], (L, N_EXPERTS, D_FF, D_MODEL), D_FF ** -0.5),
        'b_down': nrm(ks[28], (L, N_EXPERTS, D_MODEL), 0.01),
        'g_final': gain(ks[29], (D_MODEL,)),
    }


def reference(x, c, ctx, c_ctx, w_mod, b_mod, g_mix, w_in, lam_q1, lam_k1, lam_q2, lam_k2, g_subln, rpb,
              g_q_a, w_q_b, g_kv_a, w_kv_b, w_br_a, w_br_b, w_br_c, w_out, g_ffn, w_router, b_router,
              w_gate_up, b_gate_up, w_down, b_down, g_final):
    n = x.shape[1]
    rope_a = axial_tables(n, A_HEAD_DIM, x.dtype)
    rope_c = axial_tables(n, C_ROPE, x.dtype)
    scale_a = A_HEAD_DIM ** -0.5
    scale_b = B_HEAD_DIM ** -0.5
    scale_c = (C_NOPE + C_ROPE) ** -0.5
    xc = ctx
    for l in range(DEPTH):
        last = l == DEPTH - 1
        lam_init = 0.8 - 0.6 * math.exp(-0.3 * l)
        lam = (jnp.exp(jnp.sum(lam_q1[l].astype(jnp.float32) * lam_k1[l].astype(jnp.float32)))
               - jnp.exp(jnp.sum(lam_q2[l].astype(jnp.float32) * lam_k2[l].astype(jnp.float32))) + lam_init)
        mod = (jax.nn.silu(c) @ w_mod[l] + b_mod[l])[:, None, :]
        mod_c = jax.nn.silu(c_ctx) @ w_mod[l] + b_mod[l]
        sh1, sc1, gt1, sh2, sc2, gt2 = jnp.split(mod, N_MOD, axis=-1)
        csh1, csc1, cgt1, csh2, csc2, cgt2 = jnp.split(mod_c, N_MOD, axis=-1)
        h = modulate(x, g_mix[l], sh1, sc1)
        hc = modulate(xc, g_mix[l], csh1, csc1)
        qa, ka, va, qb, kb, vb, cq, ckv, ckr, gates = jnp.split(h @ w_in[l], IN_SPLITS, axis=-1)
        qa_c, ka_c, va_c, qb_c, kb_c, vb_c, cq_c, ckv_c, ckr_c, gates_c = jnp.split(hc @ w_in[l], IN_SPLITS, axis=-1)
        qA, kA, vA = diff_qkv(qa, ka, va, rope_a)
        qAc, kAc, vAc = diff_qkv(qa_c, ka_c, va_c, None)
        oA = blocked_diff_attention(qA, jnp.concatenate([kA, kAc], axis=1), jnp.concatenate([vA, vAc], axis=1), lam, scale_a)
        qB, kB, vB = heads(qb, B_HEADS), heads(kb, B_HEADS), heads(vb, B_HEADS)
        qBc, kBc, vBc = heads(qb_c, B_HEADS), heads(kb_c, B_HEADS), heads(vb_c, B_HEADS)
        oB = neighbourhood_attention(qB, kB, vB, kBc, vBc, rpb[l])
        qC, kC, vC = mla_qkv(cq, ckv, ckr, g_q_a[l], w_q_b[l], g_kv_a[l], w_kv_b[l], rope_c)
        qCc, kCc, vCc = mla_qkv(cq_c, ckv_c, ckr_c, g_q_a[l], w_q_b[l], g_kv_a[l], w_kv_b[l], None)
        oC = blocked_attention(qC, jnp.concatenate([kC, kCc], axis=1), jnp.concatenate([vC, vCc], axis=1), scale_c)
        y = merge_branches(diff_post(oA, g_subln[l], lam_init), oB.reshape(oB.shape[:2] + (B_WIDTH,)),
                           oC.reshape(oC.shape[:2] + (C_WIDTH,)), gates, w_br_a[l], w_br_b[l], w_br_c[l], w_out[l])
        x = x + gt1 * y
        x = x + gt2 * moe_ffn(modulate(x, g_ffn[l], sh2, sc2), w_router[l], b_router[l],
                              w_gate_up[l], b_gate_up[l], w_down[l], b_down[l])
        if not last:
            oAc = blocked_diff_attention(qAc, kAc, vAc, lam, scale_a)
            oBc = blocked_attention(qBc, kBc, vBc, scale_b)
            oCc = blocked_attention(qCc, kCc, vCc, scale_c)
            yc = merge_branches(diff_post(oAc, g_subln[l], lam_init), oBc.reshape(oBc.shape[:2] + (B_WIDTH,)),
                                oCc.reshape(oCc.shape[:2] + (C_WIDTH,)), gates_c, w_br_a[l], w_br_b[l], w_br_c[l], w_out[l])
            xc = xc + cgt1 * yc
            xc = xc + cgt2 * moe_ffn(modulate(xc, g_ffn[l], csh2, csc2), w_router[l], b_router[l],
                                     w_gate_up[l], b_gate_up[l], w_down[l], b_down[l])
    return rmsnorm(x, g_final)
```

```python
import math
from contextlib import ExitStack
import numpy as np
import concourse.bass as bass
import concourse.mybir as mybir
from concourse.bass_utils import run_bass_kernel_spmd

F32 = mybir.dt.float32
BF16 = mybir.dt.bfloat16
AF = mybir.ActivationFunctionType
ALU = mybir.AluOpType
AX = mybir.AxisListType

D = 1024
KC = 8
GRID_W = 64
A_HEADS, A_HD = 4, 64
B_HEADS = 8
C_HEADS, C_NOPE, C_ROPE, C_V = 8, 64, 32, 64
C_QR, C_KVR = 384, 256
EPS = 1e-6
NEG = -30000.0
LIM = 7.0
ALPHA = 1.702
FM_COLS = 3136
TM_COLS = 1664
W1_COLS = FM_COLS + TM_COLS


class Cfg:
    def __init__(self, t_own=4096, ctx=256, n_exp=32, depth=2):
        self.T_OWN = t_own
        self.T_ALL = 2 * t_own
        self.L = ctx
        self.T_EXT = self.T_ALL + ctx
        self.NE = n_exp
        self.DEPTH = depth
        self.ROWS_CORE = t_own // GRID_W
        self.ROWS_ALL = 2 * self.ROWS_CORE


class Sem:
    def __init__(self, h):
        self.h = h
        self.count = 0


class Buf:
    __slots__ = ("name", "w", "r")

    def __init__(self, name=""):
        self.name = name
        self.w = None
        self.r = {}


class Eng:
    def __init__(self, kb, name, e):
        self.name = name
        self.e = e
        self.sem = kb.new_sem("e_" + name)
        self.waited = {}


class KB:
    def __init__(self, nc):
        self.nc = nc
        self.nsem = 0
        self.pe = Eng(self, "pe", nc.tensor)
        self.act = Eng(self, "act", nc.scalar)
        self.dve = Eng(self, "dve", nc.vector)
        self.pool = Eng(self, "pool", nc.gpsimd)
        self.sp = Eng(self, "sp", nc.sync)
        self.dpool = {}
        for q in (self.sp, self.pool, self.act):
            self.dpool[q.name] = [self.new_sem("d_%s%d" % (q.name, i)) for i in range(24)]
        self.drr = {"sp": 0, "pool": 0, "act": 0}
        self.dram_bufs = {}
        self.out_events = []

    def new_sem(self, name):
        self.nsem += 1
        return Sem(self.nc.semaphore(name).__enter__())

    def dbuf(self, key):
        b = self.dram_bufs.get(key)
        if b is None:
            b = Buf(str(key))
            self.dram_bufs[key] = b
        return b

    def _wait(self, E, reads, writes):
        deps = {}
        for b in reads:
            if b.w is not None:
                s, v = b.w
                if deps.get(s, 0) < v:
                    deps[s] = v
        for b in writes:
            if b.w is not None:
                s, v = b.w
                if deps.get(s, 0) < v:
                    deps[s] = v
            for s, v in b.r.items():
                if deps.get(s, 0) < v:
                    deps[s] = v
        for s, v in deps.items():
            if s is E.sem and E is self.pe:
                continue
            if E.waited.get(s, 0) < v:
                E.e.wait_ge(s.h, v)
                E.waited[s] = v

    def op(self, E, reads, writes, fn):
        self._wait(E, reads, writes)
        ins = fn()
        E.sem.count += 1
        ins.then_inc(E.sem.h, 1)
        v = E.sem.count
        for b in reads:
            b.r[E.sem] = v
        for b in writes:
            b.w = (E.sem, v)
            b.r = {}
        return ins

    def dma(self, Q, out, in_, reads, writes, is_output=False):
        self._wait(Q, reads, writes)
        pool = self.dpool[Q.name]
        i = self.drr[Q.name]
        self.drr[Q.name] = (i + 1) % len(pool)
        ds = pool[i]
        if ds.count > 0 and Q.waited.get(ds, 0) < ds.count:
            Q.e.wait_ge(ds.h, ds.count)
            Q.waited[ds] = ds.count
        Q.e.dma_start(out=out, in_=in_).then_inc(ds.h, 16)
        ds.count += 16
        v = ds.count
        for b in reads:
            b.r[ds] = v
        for b in writes:
            b.w = (ds, v)
            b.r = {}
        if is_output:
            self.out_events.append((ds, v))

    def barrier(self):
        sems = [e.sem for e in (self.pe, self.act, self.dve, self.pool, self.sp)]
        for p in self.dpool.values():
            sems.extend(p)
        for E in (self.pe, self.act, self.dve, self.pool, self.sp):
            for s in sems:
                if s.count > 0 and E.waited.get(s, 0) < s.count and not (s is E.sem):
                    E.e.wait_ge(s.h, s.count)
                    E.waited[s] = s.count

    def finish(self):
        for b in self.dram_bufs.values():
            if b.w is not None:
                self.out_events.append(b.w)
        for s, v in self.out_events:
            if self.sp.waited.get(s, 0) < v:
                self.sp.e.wait_ge(s.h, v)
                self.sp.waited[s] = v


class Pool8:
    def __init__(self, items):
        self.items = items
        self.i = 0

    def get(self):
        it = self.items[self.i]
        self.i = (self.i + 1) % len(self.items)
        return it


def _sb(es, nc, name, shape, dt):
    t = es.enter_context(nc.sbuf_tensor(name, shape, dt))
    return t, Buf(name)


def _ps(es, nc, name, shape, dt):
    t = es.enter_context(nc.psum_tensor(name, shape, dt))
    return t, Buf(name)


def build_program(cfg, layer, last, upto=99, dump=()):
    nc = bass.Bass("TRN2", target_bir_lowering=False)
    kb = KB(nc)
    T_OWN, T_ALL, L, T_EXT, NE = cfg.T_OWN, cfg.T_ALL, cfg.L, cfg.T_EXT, cfg.NE
    T_Q = T_OWN + (0 if last else L)
    lam_init = 0.8 - 0.6 * math.exp(-0.3 * layer)

    def din(name, shape, dt=F32):
        return nc.dram_tensor(name, list(shape), dt, kind="ExternalInput").ap()

    def dscr(name, shape, dt):
        kind = "ExternalOutput" if name in dump else None
        if kind:
            return nc.dram_tensor(name, list(shape), dt, kind=kind).ap()
        return nc.dram_tensor(name, list(shape), dt).ap()

    IN_SHAPES = {
        "x_ext": [T_ALL, D], "ctx": [L, D], "cvec": [128, KC, 2], "w_mod": [D, 6 * D], "b_mod": [1, 6 * D],
        "gvecs": [3, D], "w1": [D, W1_COLS], "wg": [D, 3 * D], "ident": [128, 128],
        "cosA": [128, T_EXT], "sinA": [128, T_EXT], "cosC": [96, T_EXT], "sinC": [96, T_EXT],
        "lam": [4, 64], "gsub": [128, 1], "gqa": [128, 3], "gkva": [128, 2],
        "wqb": [C_QR, 2 * 768], "wkvb": [C_KVR, 1024],
        "rpbT": [B_HEADS, 33, 15], "bsel": [3, 4, 33, 15], "bconst": [33, 4096],
        "wbr": [3, 512, D], "wout": [D, D], "wr": [D, 32], "brt": [1, 32],
        "wgu": [NE, D, 2 * D], "bgu": [128, NE, 16], "wdn": [NE, D, D], "bdn": [NE, D],
    }
    _ins = {}

    def IN(name):
        if name not in _ins:
            _ins[name] = din(name, IN_SHAPES[name])
        return _ins[name]

    if last:
        out_x = nc.dram_tensor("out_x", [T_OWN, D], F32, kind="ExternalOutput").ap()
    else:
        out_x = nc.dram_tensor("out_x", [T_Q, D], F32, kind="ExternalOutput").ap()

    modv = dscr("modv", [2, 6, D], F32)
    hT_q = dscr("hT_q", [D, T_Q], BF16)
    QA_T = dscr("QA_T", [512, T_Q], BF16)
    KA_T = dscr("KA_T", [512, T_EXT], BF16)
    VA = dscr("VA", [T_EXT, 512], BF16)
    QB_T = dscr("QB_T", [512, T_Q], BF16)
    KB_T = dscr("KB_T", [512, T_EXT], BF16)
    VB = dscr("VB", [T_EXT, 512], BF16)
    QC_T = dscr("QC_T", [768, T_Q], BF16)
    KC_T = dscr("KC_T", [768, T_EXT], BF16)
    VC = dscr("VC", [T_EXT, 512], BF16)
    YA_T = dscr("YA_T", [512, T_Q], BF16)
    YB_T = dscr("YB_T", [512, T_Q], BF16)
    YC_T = dscr("YC_T", [512, T_Q], BF16)
    x_res = dscr("x_res", [T_Q, D], F32)
    h2T = dscr("h2T", [D, T_Q], BF16)
    gw_d = dscr("gw_d", [T_Q, 32], F32)
    btab = dscr("btab", [B_HEADS, 3, 4, 16, 4096], BF16)

    pe, act, dve, pool, sp = kb.pe, kb.act, kb.dve, kb.pool, kb.sp

    es0 = ExitStack()
    ident_f, ident_f_b = _sb(es0, nc, "ident_f", [128, 128], F32)
    ident_b, ident_b_b = _sb(es0, nc, "ident_b", [128, 128], BF16)
    ones_b, ones_b_b = _sb(es0, nc, "ones_b", [128, 128], BF16)
    ones_f, ones_f_b = _sb(es0, nc, "ones_f", [128, 128], F32)
    epsc, epsc_b = _sb(es0, nc, "epsc", [128, 1], F32)
    kb.dma(sp, ident_f[:], IN("ident")[:, :], [], [ident_f_b])
    kb.dma(pool, ident_b[:], IN("ident")[:, :], [], [ident_b_b])
    kb.op(dve, [], [ones_b_b], lambda: nc.vector.memset(ones_b[:], 1.0))
    kb.op(dve, [], [ones_f_b], lambda: nc.vector.memset(ones_f[:], 1.0))
    kb.op(dve, [], [epsc_b], lambda: nc.vector.memset(epsc[:], EPS))

    def q_chunks(width):
        res = []
        for c0 in range(0, T_OWN, width):
            res.append((c0, min(width, T_OWN - c0), c0, False))
        if not last:
            for c0 in range(0, L, width):
                res.append((T_OWN + c0, min(width, L - c0), T_ALL + c0, True))
        return res

    def phase0():
        with ExitStack() as es:
            cv, cv_b = _sb(es, nc, "cv", [128, KC, 2], F32)
            sc_, sc_b = _sb(es, nc, "silc", [128, KC, 2], F32)
            mod2, mod2_b = _sb(es, nc, "mod2", [2, 6 * D], F32)
            bm2, bm2_b = _sb(es, nc, "bm2", [2, 6 * D], F32)
            g2, g2_b = _sb(es, nc, "g2", [2, 2, D], F32)
            wts = [_sb(es, nc, "wm%d" % i, [128, KC, 512], F32) for i in range(2)]
            pss = [_ps(es, nc, "p0ps%d" % i, [128, 512], F32) for i in range(2)]
            kb.dma(sp, cv[:], IN("cvec")[:, :, :], [], [cv_b])
            kb.dma(sp, bm2[:], IN("b_mod").partition_broadcast(2), [], [bm2_b])
            for i in range(2):
                kb.dma(sp, g2[:, i, :], IN("gvecs")[i:i + 1, :].partition_broadcast(2), [], [g2_b])
            kb.op(act, [cv_b], [sc_b], lambda: nc.scalar.activation(out=sc_[:], in_=cv[:], func=AF.Silu))
            for nb in range(12):
                wt, wt_b = wts[nb % 2]
                ps, ps_b = pss[nb % 2]
                kb.dma(sp, wt[:], IN("w_mod")[:, nb * 512:(nb + 1) * 512].rearrange("(k p) n -> p k n", p=128), [], [wt_b])
                for k in range(KC):
                    kb.op(pe, [sc_b, wt_b], [ps_b],
                          lambda k=k: nc.tensor.matmul(ps[0:2, :], lhsT=sc_[:, k, :], rhs=wt[:, k, :],
                                                       start=(k == 0), stop=(k == KC - 1)))
                kb.op(dve, [ps_b, bm2_b], [mod2_b],
                      lambda: nc.vector.tensor_tensor(out=mod2[:, nb * 512:(nb + 1) * 512], in0=ps[0:2, :],
                                                      in1=bm2[:, nb * 512:(nb + 1) * 512], op=ALU.add))
            for (ci, gi) in ((1, 0), (4, 1)):
                kb.op(dve, [mod2_b, g2_b], [mod2_b],
                      lambda ci=ci, gi=gi: nc.vector.scalar_tensor_tensor(
                          out=mod2[:, ci * D:(ci + 1) * D], in0=mod2[:, ci * D:(ci + 1) * D], scalar=1.0,
                          in1=g2[:, gi, :], op0=ALU.add, op1=ALU.mult))
            kb.dma(sp, modv.rearrange("a s d -> a (s d)"), mod2[:], [mod2_b], [kb.dbuf("modv")])

    def load_bc(es, name, row_ap):
        n = row_ap.shape[-1]
        t, b = _sb(es, nc, name, [128, n], F32)
        kb.dma(sp, t[:], row_ap.partition_broadcast(128), [kb.dbuf("modv")], [b])
        return t, b

    def rstd_of(x_ap, xb, n, junk, junk_b, st, st_b, col):
        kb.op(act, [xb], [junk_b, st_b],
              lambda: nc.scalar.activation(out=junk, in_=x_ap, func=AF.Square, accum_out=st[:, col:col + 1]))
        kb.op(act, [st_b], [st_b],
              lambda: nc.scalar.activation(out=st[:, col + 1:col + 2], in_=st[:, col:col + 1], func=AF.Sqrt,
                                           scale=1.0 / n, bias=epsc[:, 0:1]))
        kb.op(dve, [st_b], [st_b],
              lambda: nc.vector.reciprocal(out=st[:, col + 2:col + 3], in_=st[:, col + 1:col + 2]))
        return st[:, col + 2:col + 3]

    def phase1():
        with ExitStack() as es:
            W1, W1_b = _sb(es, nc, "W1", [128, KC, W1_COLS], BF16)
            for k in range(KC):
                for c0 in range(0, W1_COLS, 1600):
                    kb.dma(pool, W1[:, k, c0:c0 + 1600], IN("w1")[k * 128:(k + 1) * 128, c0:c0 + 1600], [], [W1_b])
            Wqb, Wqb_b = _sb(es, nc, "Wqb", [128, 3, 1536], BF16)
            Wqf, Wqf_b = _sb(es, nc, "Wqf", [128, 1536], F32)
            gq, gq_b = _sb(es, nc, "gq", [128, 3], F32)
            gk, gk_b = _sb(es, nc, "gk", [128, 2], F32)
            Wkv, Wkv_b = _sb(es, nc, "Wkv", [128, 2, 1024], BF16)
            kb.dma(sp, gq[:], IN("gqa")[:, :], [], [gq_b])
            kb.dma(sp, gk[:], IN("gkva")[:, :], [], [gk_b])
            for k in range(3):
                kb.dma(sp, Wqf[:, :], IN("wqb")[k * 128:(k + 1) * 128, :], [], [Wqf_b])
                kb.op(dve, [Wqf_b, gq_b], [Wqb_b],
                      lambda k=k: nc.vector.tensor_scalar(out=Wqb[:, k, :], in0=Wqf[:, :], scalar1=gq[:, k:k + 1],
                                                          scalar2=None, op0=ALU.mult))
            for k in range(2):
                kb.dma(sp, Wqf[:, 0:1024], IN("wkvb")[k * 128:(k + 1) * 128, :], [], [Wqf_b])
                kb.op(dve, [Wqf_b, gk_b], [Wkv_b],
                      lambda k=k: nc.vector.tensor_scalar(out=Wkv[:, k, :], in0=Wqf[:, 0:1024], scalar1=gk[:, k:k + 1],
                                                          scalar2=None, op0=ALU.mult))
            gs_l, gs_l_b = load_bc(es, "gs1l", modv[0, 1:2, :])
            sh_l, sh_l_b = load_bc(es, "sh1l", modv[0, 0:1, :])
            gs_c, gs_c_b = load_bc(es, "gs1c", modv[1, 1:2, :])
            sh_c, sh_c_b = load_bc(es, "sh1c", modv[1, 0:1, :])

            xts = Pool8([_sb(es, nc, "xt%d" % i, [128, D], F32) for i in range(2)])
            tmp, tmp_b = _sb(es, nc, "p1tmp", [128, D], F32)
            junk, junk_b = _sb(es, nc, "p1junk", [128, D], BF16)
            hbs = Pool8([_sb(es, nc, "hb%d" % i, [128, D], BF16) for i in range(2)])
            st, st_b = _sb(es, nc, "p1st", [128, 16], F32)
            hTs = Pool8([_sb(es, nc, "hT%d" % i, [128, KC, 512], BF16) for i in range(2)])
            cst = Pool8([_sb(es, nc, "cst%d" % i, [128, 4, 512], F32) for i in range(1)])
            fm_st = Pool8([_sb(es, nc, "fmst%d" % i, [128, 4, 512], BF16) for i in range(2)])
            tm_st = Pool8([_sb(es, nc, "tmst%d" % i, [128, 512], BF16) for i in range(3)])
            r1, r1_b = _sb(es, nc, "r1", [128, 512], F32)
            r2, r2_b = _sb(es, nc, "r2", [128, 512], F32)
            cqn = Pool8([_sb(es, nc, "cqn%d" % i, [128, 384], BF16) for i in range(2)])
            cqnT, cqnT_b = _sb(es, nc, "cqnT", [128, 3, 512], BF16)
            ckvnT, ckvnT_b = _sb(es, nc, "ckvnT", [128, 2, 512], BF16)
            kpe, kpe_b = _sb(es, nc, "kpe", [32, 512], BF16)
            qc_st = Pool8([_sb(es, nc, "qcst%d" % i, [96, 512], BF16) for i in range(2)])
            psT, psT_b = _ps(es, nc, "p1psT", [128, 1024], BF16)
            psf = Pool8([_ps(es, nc, "p1ps%d" % i, [128, 512], F32) for i in range(7)])

            chunks = [(c0, 512, False) for c0 in range(0, T_ALL, 512)] + [(T_ALL + c0, min(512, L - c0), True) for c0 in range(0, L, 512)]
            for (e0, n, is_ctx) in chunks:
                is_q = (e0 < T_OWN) or (is_ctx and not last)
                q0 = e0 if e0 < T_OWN else (T_OWN + e0 - T_ALL)
                nt = n // 128
                gs, gs_b = (gs_c, gs_c_b) if is_ctx else (gs_l, gs_l_b)
                sh, sh_b = (sh_c, sh_c_b) if is_ctx else (sh_l, sh_l_b)
                hT, hT_b = hTs.get()
                cs, cs_b = cst.get()
                kb.dma(sp, cs[:, 0, :n], IN("cosA")[:, e0:e0 + n], [], [cs_b])
                kb.dma(sp, cs[:, 1, :n], IN("sinA")[:, e0:e0 + n], [], [cs_b])
                kb.dma(sp, cs[0:96, 2, :n], IN("cosC")[:, e0:e0 + n], [], [cs_b])
                kb.dma(sp, cs[0:96, 3, :n], IN("sinC")[:, e0:e0 + n], [], [cs_b])
                for t in range(nt):
                    xt, xt_b = xts.get()
                    src = IN("ctx")[e0 - T_ALL + t * 128: e0 - T_ALL + (t + 1) * 128, :] if is_ctx else IN("x_ext")[e0 + t * 128:e0 + (t + 1) * 128, :]
                    kb.dma(sp, xt[:], src, [], [xt_b])
                    rstd = rstd_of(xt[:], xt_b, D, junk[:], junk_b, st, st_b, 0)
                    kb.op(dve, [xt_b, st_b, gs_b], [tmp_b],
                          lambda: nc.vector.scalar_tensor_tensor(out=tmp[:], in0=xt[:], scalar=rstd, in1=gs[:],
                                                                 op0=ALU.mult, op1=ALU.mult))
                    hb, hb_b = hbs.get()
                    kb.op(pool, [tmp_b, sh_b], [hb_b],
                          lambda: nc.gpsimd.tensor_tensor(out=hb[:], in0=tmp[:], in1=sh[:], op=ALU.add))
                    for k in range(KC):
                        kb.op(pe, [hb_b, ident_b_b], [psT_b],
                              lambda k=k: nc.tensor.transpose(psT[:, k * 128:(k + 1) * 128], hb[:, k * 128:(k + 1) * 128], ident_b[:]))
                    kb.op(act, [psT_b], [hT_b],
                          lambda t=t: nc.scalar.copy(out=hT[:, :, t * 128:(t + 1) * 128],
                                                     in_=psT[:].rearrange("p (k t) -> p k t", k=KC)))
                if is_q:
                    kb.dma(sp, hT_q.rearrange("(k p) t -> p k t", p=128)[:, :, q0:q0 + n], hT[:, :, :n], [hT_b], [kb.dbuf("hT_q")])

                def fm_group(col0, nchunks, rope, dst, dcol0, dkey):
                    stg, stg_b = fm_st.get()
                    for c in range(nchunks):
                        pa, pa_b = psf.get()
                        for k in range(KC):
                            kb.op(pe, [W1_b, hT_b], [pa_b],
                                  lambda k=k: nc.tensor.matmul(pa[:, :n], lhsT=W1[:, k, col0 + c * 128: col0 + (c + 1) * 128],
                                                               rhs=hT[:, k, :n], start=(k == 0), stop=(k == KC - 1)))
                        if rope:
                            pb, pb_b = psf.get()
                            for k in range(KC):
                                kb.op(pe, [W1_b, hT_b], [pb_b],
                                      lambda k=k: nc.tensor.matmul(pb[:, :n], lhsT=W1[:, k, col0 + 512 + c * 128: col0 + 512 + (c + 1) * 128],
                                                                   rhs=hT[:, k, :n], start=(k == 0), stop=(k == KC - 1)))
                            kb.op(dve, [pa_b, cs_b], [r1_b],
                                  lambda: nc.vector.tensor_tensor(out=r1[:, :n], in0=pa[:, :n], in1=cs[:, 0, :n], op=ALU.mult))
                            kb.op(dve, [pb_b, cs_b], [r2_b],
                                  lambda: nc.vector.tensor_tensor(out=r2[:, :n], in0=pb[:, :n], in1=cs[:, 1, :n], op=ALU.mult))
                            kb.op(pool, [r1_b, r2_b], [stg_b],
                                  lambda c=c: nc.gpsimd.tensor_tensor(out=stg[:, c, :n], in0=r1[:, :n], in1=r2[:, :n], op=ALU.add))
                        else:
                            kb.op(act, [pa_b], [stg_b], lambda c=c: nc.scalar.copy(out=stg[:, c, :n], in_=pa[:, :n]))
                    kb.dma(sp, dst.rearrange("(c p) t -> p c t", p=128)[:, :, dcol0:dcol0 + n], stg[:, :nchunks, :n],
                           [stg_b], [kb.dbuf(dkey)])

                if is_q:
                    fm_group(0, 4, True, QA_T, q0, "QA_T")
                    fm_group(2048, 4, False, QB_T, q0, "QB_T")
                fm_group(1024, 4, True, KA_T, e0, "KA_T")
                fm_group(2560, 4, False, KB_T, e0, "KB_T")
                pa, pa_b = psf.get()
                pb, pb_b = psf.get()
                for k in range(KC):
                    kb.op(pe, [W1_b, hT_b], [pa_b],
                          lambda k=k: nc.tensor.matmul(pa[0:32, :n], lhsT=W1[:, k, 3072:3104], rhs=hT[:, k, :n],
                                                       start=(k == 0), stop=(k == KC - 1)))
                for k in range(KC):
                    kb.op(pe, [W1_b, hT_b], [pb_b],
                          lambda k=k: nc.tensor.matmul(pb[0:32, :n], lhsT=W1[:, k, 3104:3136], rhs=hT[:, k, :n],
                                                       start=(k == 0), stop=(k == KC - 1)))
                kb.op(dve, [pa_b, cs_b], [r1_b],
                      lambda: nc.vector.tensor_tensor(out=r1[0:32, :n], in0=pa[0:32, :n], in1=cs[0:32, 2, :n], op=ALU.mult))
                kb.op(dve, [pb_b, cs_b], [r2_b],
                      lambda: nc.vector.tensor_tensor(out=r2[0:32, :n], in0=pb[0:32, :n], in1=cs[0:32, 3, :n], op=ALU.mult))
                kb.op(pool, [r1_b, r2_b], [kpe_b],
                      lambda: nc.gpsimd.tensor_tensor(out=kpe[:, :n], in0=r1[0:32, :n], in1=r2[0:32, :n], op=ALU.add))
                for h in range(C_HEADS):
                    kb.dma(sp, KC_T[h * 96:h * 96 + 32, e0:e0 + n], kpe[:, :n], [kpe_b], [kb.dbuf("KC_T")])

                for t in range(nt):
                    ts_ = slice(t * 128, (t + 1) * 128)
                    for (col0, dst, dkey) in ((FM_COLS, VA, "VA"), (FM_COLS + 512, VB, "VB")):
                        pa, pa_b = psf.get()
                        for k in range(KC):
                            kb.op(pe, [W1_b, hT_b], [pa_b],
                                  lambda k=k: nc.tensor.matmul(pa[:, :], lhsT=hT[:, k, ts_], rhs=W1[:, k, col0:col0 + 512],
                                                               start=(k == 0), stop=(k == KC - 1)))
                        stg, stg_b = tm_st.get()
                        kb.op(act, [pa_b], [stg_b], lambda: nc.scalar.copy(out=stg[:], in_=pa[:, :]))
                        kb.dma(sp, dst[e0 + t * 128:e0 + (t + 1) * 128, :], stg[:], [stg_b], [kb.dbuf(dkey)])
                    for (col0, w, dstT, dstT_b, nk, scol) in ((FM_COLS + 1024, 384, cqnT, cqnT_b, 3, 4), (FM_COLS + 1408, 256, ckvnT, ckvnT_b, 2, 8)):
                        if w == 384 and not is_q:
                            continue
                        pa, pa_b = psf.get()
                        for k in range(KC):
                            kb.op(pe, [W1_b, hT_b], [pa_b],
                                  lambda k=k: nc.tensor.matmul(pa[:, :w], lhsT=hT[:, k, ts_], rhs=W1[:, k, col0:col0 + w],
                                                               start=(k == 0), stop=(k == KC - 1)))
                        rs_ = rstd_of(pa[:, :w], pa_b, w, junk[:, :w], junk_b, st, st_b, scol)
                        cn, cn_b = cqn.get()
                        kb.op(act, [pa_b, st_b], [cn_b],
                              lambda: nc.scalar.activation(out=cn[:, :w], in_=pa[:, :w], func=AF.Copy, scale=rs_))
                        for k in range(nk):
                            kb.op(pe, [cn_b, ident_b_b], [psT_b],
                                  lambda k=k: nc.tensor.transpose(psT[:, k * 128:(k + 1) * 128], cn[:, k * 128:(k + 1) * 128], ident_b[:]))
                        kb.op(act, [psT_b], [dstT_b],
                              lambda: nc.scalar.copy(out=dstT[:, :, ts_], in_=psT[:, 0:nk * 128].rearrange("p (k t) -> p k t", k=nk)))
                    pa, pa_b = psf.get()
                    for k in range(2):
                        kb.op(pe, [Wkv_b, ckvnT_b], [pa_b],
                              lambda k=k: nc.tensor.matmul(pa[:, :], lhsT=ckvnT[:, k, ts_], rhs=Wkv[:, k, 512:1024],
                                                           start=(k == 0), stop=(k == 1)))
                    stg, stg_b = tm_st.get()
                    kb.op(act, [pa_b], [stg_b], lambda: nc.scalar.copy(out=stg[:], in_=pa[:, :]))
                    kb.dma(sp, VC[e0 + t * 128:e0 + (t + 1) * 128, :], stg[:], [stg_b], [kb.dbuf("VC")])
                for h in range(C_HEADS):
                    pa, pa_b = psf.get()
                    for k in range(2):
                        kb.op(pe, [Wkv_b, ckvnT_b], [pa_b],
                              lambda k=k: nc.tensor.matmul(pa[0:64, :n], lhsT=Wkv[:, k, h * 64:(h + 1) * 64], rhs=ckvnT[:, k, :n],
                                                           start=(k == 0), stop=(k == 1)))
                    stg, stg_b = qc_st.get()
                    kb.op(act, [pa_b], [stg_b], lambda: nc.scalar.copy(out=stg[0:64, :n], in_=pa[0:64, :n]))
                    kb.dma(sp, KC_T[h * 96 + 32:h * 96 + 96, e0:e0 + n], stg[0:64, :n], [stg_b], [kb.dbuf("KC_T")])
                if is_q:
                    for h in range(C_HEADS):
                        pa, pa_b = psf.get()
                        pb, pb_b = psf.get()
                        for k in range(3):
                            kb.op(pe, [Wqb_b, cqnT_b], [pa_b],
                                  lambda k=k: nc.tensor.matmul(pa[0:96, :n], lhsT=Wqb[:, k, h * 96:(h + 1) * 96], rhs=cqnT[:, k, :n],
                                                               start=(k == 0), stop=(k == 2)))
                        for k in range(3):
                            kb.op(pe, [Wqb_b, cqnT_b], [pb_b],
                                  lambda k=k: nc.tensor.matmul(pb[0:96, :n], lhsT=Wqb[:, k, 768 + h * 96:768 + (h + 1) * 96], rhs=cqnT[:, k, :n],
                                                               start=(k == 0), stop=(k == 2)))
                        kb.op(dve, [pa_b, cs_b], [r1_b],
                              lambda: nc.vector.tensor_tensor(out=r1[0:96, :n], in0=pa[0:96, :n], in1=cs[0:96, 2, :n], op=ALU.mult))
                        kb.op(dve, [pb_b, cs_b], [r2_b],
                              lambda: nc.vector.tensor_tensor(out=r2[0:96, :n], in0=pb[0:96, :n], in1=cs[0:96, 3, :n], op=ALU.mult))
                        stg, stg_b = qc_st.get()
                        kb.op(pool, [r1_b, r2_b], [stg_b],
                              lambda: nc.gpsimd.tensor_tensor(out=stg[0:96, :n], in0=r1[0:96, :n], in1=r2[0:96, :n], op=ALU.add))
                        kb.dma(sp, QC_T[h * 96:(h + 1) * 96, q0:q0 + n], stg[0:96, :n], [stg_b], [kb.dbuf("QC_T")])

    def phase_attn(kind):
        n_kt = T_EXT // 128
        ctx_kts = list(range(T_ALL // 128, n_kt))
        if kind == "A":
            NH, ROWS, DV, QT_src, KT_src, V_src, Y_dst, scale = A_HEADS, 128, 128, QA_T, KA_T, VA, YA_T, A_HD ** -0.5
            comps = [0, 1]
        else:
            NH, ROWS, DV, QT_src, KT_src, V_src, Y_dst, scale = C_HEADS, 96, 64, QC_T, KC_T, VC, YC_T, 96 ** -0.5
            comps = [0]
        nm = kind
        with ExitStack() as es:
            Vall, Vall_b = _sb(es, nc, "Vall" + nm, [128, n_kt, 512], BF16)
            vsrc = V_src.rearrange("(kt p) e -> p kt e", p=128)
            for k0 in range(0, n_kt, 8):
                k1 = min(n_kt, k0 + 8)
                kb.dma(sp, Vall[:, k0:k1, :], vsrc[:, k0:k1, :], [kb.dbuf("V" + nm)], [Vall_b])
            KTs = Pool8([_sb(es, nc, "KT%s%d" % (nm, i), [ROWS, T_EXT], BF16) for i in range(2)])
            QTs = Pool8([_sb(es, nc, "QT%s%d" % (nm, i), [ROWS, T_Q], BF16) for i in range(2)])
            PTs = Pool8([_sb(es, nc, "PT%s%d" % (nm, i), [128, 512], BF16) for i in range(4)])
            psS = [Pool8([_ps(es, nc, "psS%s%d_%d" % (nm, c, i), [128, 512], F32) for i in range(2 if kind == "A" else 3)]) for c in comps]
            psO = [_ps(es, nc, "psO%s%d" % (nm, c), [128, 512], F32) for c in range(2)]
            psM = [_ps(es, nc, "psM%s%d" % (nm, c), [128, 512], F32) for c in range(2)]
            rs_, rs_b = _sb(es, nc, "ars" + nm, [128, 512], F32)
            oc = [_sb(es, nc, "aoc%s%d" % (nm, c), [128, 512], F32) for c in range(2)]
            osq, osq_b = _sb(es, nc, "aosq" + nm, [128, 512], BF16)
            sd, sd_b = _sb(es, nc, "asd" + nm, [128, 512], F32)
            ysts = Pool8([_sb(es, nc, "ayst%s%d" % (nm, i), [128, 512], BF16) for i in range(2)])
            if kind == "A":
                lamt, lamt_b = _sb(es, nc, "lamt", [128, 4, 64], F32)
                lt, lt_b = _sb(es, nc, "lamtmp", [128, 2, 64], F32)
                ls, ls_b = _sb(es, nc, "lams", [128, 8], F32)
                gsb, gsb_b = _sb(es, nc, "gsubs", [128, 2], F32)
                kb.dma(sp, lamt[:].rearrange("p a d -> p (a d)"), IN("lam").rearrange("a d -> (a d)").partition_broadcast(128), [], [lamt_b])
                kb.dma(sp, gsb[:, 0:1], IN("gsub")[:, :], [], [gsb_b])
                for i in range(2):
                    kb.op(dve, [lamt_b], [lt_b],
                          lambda i=i: nc.vector.tensor_tensor(out=lt[:, i, :], in0=lamt[:, 2 * i, :], in1=lamt[:, 2 * i + 1, :], op=ALU.mult))
                    kb.op(dve, [lt_b], [ls_b],
                          lambda i=i: nc.vector.reduce_sum(out=ls[:, i:i + 1], in_=lt[:, i, :], axis=AX.X))
                kb.op(act, [ls_b], [ls_b], lambda: nc.scalar.activation(out=ls[:, 2:4], in_=ls[:, 0:2], func=AF.Exp))
                kb.op(dve, [ls_b], [ls_b],
                      lambda: nc.vector.tensor_tensor(out=ls[:, 4:5], in0=ls[:, 3:4], in1=ls[:, 2:3], op=ALU.subtract))
                kb.op(dve, [ls_b], [ls_b],
                      lambda: nc.vector.tensor_scalar(out=ls[:, 5:6], in0=ls[:, 4:5], scalar1=-lam_init, scalar2=None, op0=ALU.add))
                kb.op(dve, [gsb_b], [gsb_b],
                      lambda: nc.vector.tensor_scalar(out=gsb[:, 1:2], in0=gsb[:, 0:1], scalar1=1.0 - lam_init, scalar2=None, op0=ALU.mult))
            oi = 0
            for h in range(NH):
                KT, KT_b = KTs.get()
                QT, QT_b = QTs.get()
                kb.dma(sp, KT[:], KT_src[h * ROWS:(h + 1) * ROWS, :], [kb.dbuf("KT" + nm)], [KT_b])
                kb.dma(sp, QT[:], QT_src[h * ROWS:(h + 1) * ROWS, :], [kb.dbuf("QT" + nm)], [QT_b])
                for (q0, n, ext0, is_ctx) in q_chunks(512):
                    kts = ctx_kts if is_ctx else list(range(n_kt))
                    nk = len(kts)
                    if kind == "A":
                        pO = [psO[0], psO[1]]
                        pM = [psM[0], psM[1]]
                    else:
                        pO = [psO[oi % 2]]
                        pM = [psM[oi % 2]]
                        oi += 1
                    S_t = {}

                    def issue_mm1(i):
                        kt = kts[i]
                        for c in comps:
                            ps, ps_b = psS[c].get()
                            r0 = 64 * c if kind == "A" else 0
                            r1 = r0 + 64 if kind == "A" else 96
                            kb.op(pe, [KT_b, QT_b], [ps_b],
                                  lambda: nc.tensor.matmul(ps[:, :n], lhsT=KT[r0:r1, kt * 128:(kt + 1) * 128], rhs=QT[r0:r1, q0:q0 + n],
                                                           start=True, stop=True))
                            S_t[(i, c)] = (ps, ps_b)

                    issue_mm1(0)
                    for i in range(nk):
                        kt = kts[i]
                        if i + 1 < nk:
                            issue_mm1(i + 1)
                        pts = []
                        for c in comps:
                            ps, ps_b = S_t.pop((i, c))
                            PT, PT_b = PTs.get()
                            kb.op(act, [ps_b], [PT_b],
                                  lambda: nc.scalar.activation(out=PT[:, :n], in_=ps[:, :n], func=AF.Exp, scale=scale))
                            pts.append((PT, PT_b))
                        for ci, c in enumerate(comps):
                            PT, PT_b = pts[ci]
                            po, po_b = pO[ci]
                            pm, pm_b = pM[ci]
                            kb.op(pe, [Vall_b, PT_b], [po_b],
                                  lambda: nc.tensor.matmul(po[0:DV, :n], lhsT=Vall[:, kt, h * DV:(h + 1) * DV], rhs=PT[:, :n],
                                                           start=(i == 0), stop=(i == nk - 1)))
                            kb.op(pe, [ones_b_b, PT_b], [pm_b],
                                  lambda: nc.tensor.matmul(pm[0:DV, :n], lhsT=ones_b[:, 0:DV], rhs=PT[:, :n],
                                                           start=(i == 0), stop=(i == nk - 1)))
                    yst, yst_b = ysts.get()
                    if kind == "C":
                        po, po_b = pO[0]
                        pm, pm_b = pM[0]
                        kb.op(dve, [pm_b], [rs_b], lambda: nc.vector.reciprocal(out=rs_[0:DV, :n], in_=pm[0:DV, :n]))
                        kb.op(dve, [po_b, rs_b], [yst_b],
                              lambda: nc.vector.tensor_tensor(out=yst[0:DV, :n], in0=po[0:DV, :n], in1=rs_[0:DV, :n], op=ALU.mult))
                        kb.dma(sp, Y_dst[h * DV:(h + 1) * DV, q0:q0 + n], yst[0:DV, :n], [yst_b], [kb.dbuf("Y" + nm)])
                    else:
                        for c in comps:
                            po, po_b = pO[c]
                            pm, pm_b = pM[c]
                            o_, o_b = oc[c]
                            kb.op(dve, [pm_b], [rs_b], lambda: nc.vector.reciprocal(out=rs_[:, :n], in_=pm[:, :n]))
                            kb.op(dve, [po_b, rs_b], [o_b],
                                  lambda: nc.vector.tensor_tensor(out=o_[:, :n], in0=po[:, :n], in1=rs_[:, :n], op=ALU.mult))
                        o1, o1_b = oc[0]
                        o2, o2_b = oc[1]
                        kb.op(dve, [o1_b, o2_b, ls_b], [o1_b],
                              lambda: nc.vector.scalar_tensor_tensor(out=o1[:, :n], in0=o2[:, :n], scalar=ls[:, 5:6], in1=o1[:, :n],
                                                                     op0=ALU.mult, op1=ALU.add))
                        kb.op(pool, [o1_b], [osq_b],
                              lambda: nc.gpsimd.tensor_tensor(out=osq[:, :n], in0=o1[:, :n], in1=o1[:, :n], op=ALU.mult))
                        pq, pq_b = psS[0].get()
                        kb.op(pe, [ones_b_b, osq_b], [pq_b],
                              lambda: nc.tensor.matmul(pq[:, :n], lhsT=ones_b[:, :], rhs=osq[:, :n], start=True, stop=True))
                        kb.op(act, [pq_b], [sd_b],
                              lambda: nc.scalar.activation(out=sd[:, :n], in_=pq[:, :n], func=AF.Sqrt, scale=1.0 / 128, bias=epsc[:, 0:1]))
                        kb.op(dve, [sd_b], [sd_b], lambda: nc.vector.reciprocal(out=sd[:, :n], in_=sd[:, :n]))
                        kb.op(dve, [o1_b, sd_b, gsb_b], [yst_b],
                              lambda: nc.vector.scalar_tensor_tensor(out=yst[:, :n], in0=o1[:, :n], scalar=gsb[:, 1:2], in1=sd[:, :n],
                                                                     op0=ALU.mult, op1=ALU.mult))
                        kb.dma(sp, Y_dst[h * 128:(h + 1) * 128, q0:q0 + n], yst[:, :n], [yst_b], [kb.dbuf("Y" + nm)])

    def phase_attn_B():
        NT = cfg.ROWS_CORE // 4
        LOC = 256 + T_OWN + 256 + L
        n_lt = LOC // 128
        scale = 64 ** -0.5
        with ExitStack() as es:
            rp, rp_b = _sb(es, nc, "rp", [33, B_HEADS, 15], F32)
            bs, bs_b = _sb(es, nc, "bs", [33, 12, 15], F32)
            bcn, bcn_b = _sb(es, nc, "bcn", [33, 4096], BF16)
            lw = Pool8([_sb(es, nc, "blw%d" % i, [33, 16], BF16) for i in range(3)])
            tst = Pool8([_sb(es, nc, "btst%d" % i, [16, 4096], BF16) for i in range(2)])
            pss = Pool8([_ps(es, nc, "bps%d" % i, [128, 512], F32) for i in range(4)])
            kb.dma(sp, rp[:], IN("rpbT").rearrange("h r i -> r h i"), [], [rp_b])
            kb.dma(sp, bs[:], IN("bsel").rearrange("c b r i -> r (c b) i"), [], [bs_b])
            kb.dma(pool, bcn[:], IN("bconst")[:, :], [], [bcn_b])
            for tt in tst.items:
                kb.op(dve, [], [tt[1]], lambda tt=tt: nc.vector.memset(tt[0][:], 0.0))
            for h in range(B_HEADS):
                for cb in range(12):
                    l_, l_b = lw.get()
                    kb.op(dve, [rp_b, bs_b], [l_b],
                          lambda: nc.vector.tensor_tensor(out=l_[:, 0:15], in0=rp[:, h, :], in1=bs[:, cb, :], op=ALU.mult))
                    st_, st_b2 = tst.get()
                    for sl in range(8):
                        ps, ps_b = pss.get()
                        kb.op(pe, [l_b, bcn_b], [ps_b],
                              lambda: nc.tensor.matmul(ps[0:15, :], lhsT=l_[:, 0:15], rhs=bcn[:, sl * 512:(sl + 1) * 512], start=True, stop=True))
                        kb.op(act, [ps_b], [st_b2], lambda: nc.scalar.copy(out=st_[0:15, sl * 512:(sl + 1) * 512], in_=ps[0:15, :]))
                    kb.dma(sp, btab[h, cb // 4, cb % 4, :, :], st_[:, :], [st_b2], [kb.dbuf("btab")])
        kb.barrier()
        with ExitStack() as es:
            Vall, Vall_b = _sb(es, nc, "VallB", [128, n_lt, 512], BF16)
            vsrc = VB.rearrange("(kt p) e -> p kt e", p=128)
            segs = [(0, T_ALL - 256, 256), (256, 0, T_OWN), (256 + T_OWN, T_OWN, 256), (512 + T_OWN, T_ALL, L)]
            for (l0, e0, ln) in segs:
                for k0 in range(0, ln // 128, 8):
                    k1 = min(ln // 128, k0 + 8)
                    kb.dma(sp, Vall[:, l0 // 128 + k0:l0 // 128 + k1, :], vsrc[:, e0 // 128 + k0:e0 // 128 + k1, :], [kb.dbuf("VB")], [Vall_b])
            KTs = Pool8([_sb(es, nc, "KTB%d" % i, [64, LOC], BF16) for i in range(2)])
            QTs = Pool8([_sb(es, nc, "QTB%d" % i, [64, T_Q], BF16) for i in range(2)])
            BTs = Pool8([_sb(es, nc, "BTB%d" % i, [128, 3, 6, 256], BF16) for i in range(2)])
            PTs = Pool8([_sb(es, nc, "PTB%d" % i, [128, 256], BF16) for i in range(4)])
            psS = Pool8([_ps(es, nc, "psSB%d" % i, [128, 512], F32) for i in range(3)])
            psO = Pool8([_ps(es, nc, "psOB%d" % i, [128, 512], F32) for i in range(2)])
            psM = Pool8([_ps(es, nc, "psMB%d" % i, [128, 512], F32) for i in range(2)])
            rs_, rs_b = _sb(es, nc, "brs", [64, 256], F32)
            ysts = Pool8([_sb(es, nc, "byst%d" % i, [64, 256], BF16) for i in range(2)])
            for h in range(B_HEADS):
                KT, KT_b = KTs.get()
                QT, QT_b = QTs.get()
                BT, BT_b = BTs.get()
                for (l0, e0, ln) in segs:
                    kb.dma(sp, KT[:, l0:l0 + ln], KB_T[h * 64:(h + 1) * 64, e0:e0 + ln], [kb.dbuf("KB_T")], [KT_b])
                kb.dma(sp, QT[:], QB_T[h * 64:(h + 1) * 64, :], [kb.dbuf("QB_T")], [QT_b])
                for cls in range(3):
                    flat = btab[h, cls].rearrange("b i n -> (b i) n")
                    for jt in range(6):
                        for a in range(2):
                            i0 = 11 - (2 * jt + a)
                            src = flat[i0:i0 + 52:17, :].rearrange("b (kc qc) -> kc b qc", qc=64)
                            kb.dma(sp, BT[a * 64:(a + 1) * 64, cls, jt, :].rearrange("p (b qc) -> p b qc", qc=64), src,
                                   [kb.dbuf("btab")], [BT_b])
                work = []
                for t in range(NT):
                    cls = 0 if t == 0 else (2 if t == NT - 1 else 1)
                    tiles = [((256 * t + 128 * jt) // 128, (cls, jt)) for jt in range(6)] + [((512 + T_OWN) // 128 + i, None) for i in range(L // 128)]
                    work.append((t * 256, 256, tiles))
                if not last:
                    work.append((T_OWN, L, [((512 + T_OWN) // 128 + i, None) for i in range(L // 128)]))
                for (q0, n, tiles) in work:
                    po, po_b = psO.get()
                    pm, pm_b = psM.get()
                    nk = len(tiles)
                    for i, (lt_, bias) in enumerate(tiles):
                        ps, ps_b = psS.get()
                        kb.op(pe, [KT_b, QT_b], [ps_b],
                              lambda: nc.tensor.matmul(ps[:, :n], lhsT=KT[:, lt_ * 128:(lt_ + 1) * 128], rhs=QT[:, q0:q0 + n],
                                                       start=True, stop=(bias is None)))
                        if bias is not None:
                            kb.op(pe, [ident_b_b, BT_b], [ps_b],
                                  lambda: nc.tensor.matmul(ps[:, :n], lhsT=ident_b[:, :], rhs=BT[:, bias[0], bias[1], :n],
                                                           start=False, stop=True))
                        PT, PT_b = PTs.get()
                        kb.op(act, [ps_b], [PT_b],
                              lambda: nc.scalar.activation(out=PT[:, :n], in_=ps[:, :n], func=AF.Exp, scale=scale))
                        kb.op(pe, [Vall_b, PT_b], [po_b],
                              lambda: nc.tensor.matmul(po[0:64, :n], lhsT=Vall[:, lt_, h * 64:(h + 1) * 64], rhs=PT[:, :n],
                                                       start=(i == 0), stop=(i == nk - 1)))
                        kb.op(pe, [ones_b_b, PT_b], [pm_b],
                              lambda: nc.tensor.matmul(pm[0:64, :n], lhsT=ones_b[:, 0:64], rhs=PT[:, :n],
                                                       start=(i == 0), stop=(i == nk - 1)))
                    yst, yst_b = ysts.get()
                    kb.op(dve, [pm_b], [rs_b], lambda: nc.vector.reciprocal(out=rs_[:, :n], in_=pm[0:64, :n]))
                    kb.op(dve, [po_b, rs_b], [yst_b],
                          lambda: nc.vector.tensor_tensor(out=yst[:, :n], in0=po[0:64, :n], in1=rs_[:, :n], op=ALU.mult))
                    kb.dma(sp, YB_T[h * 64:(h + 1) * 64, q0:q0 + n], yst[:, :n], [yst_b], [kb.dbuf("YB_T")])

    def phase_merge():
        with ExitStack() as es:
            Wg, Wg_b = _sb(es, nc, "Wg", [128, KC, 3 * D], BF16)
            for k in range(KC):
                for c0 in range(0, 3 * D, 1536):
                    kb.dma(pool, Wg[:, k, c0:c0 + 1536], IN("wg")[k * 128:(k + 1) * 128, c0:c0 + 1536], [], [Wg_b])
            Wbr, Wbr_b = _sb(es, nc, "Wbr", [128, 3, 4, D], BF16)
            for i in range(3):
                for k in range(4):
                    kb.dma(pool, Wbr[:, i, k, :], IN("wbr")[i, k * 128:(k + 1) * 128, :], [], [Wbr_b])
            Wo, Wo_b = _sb(es, nc, "Wo", [128, KC, D], BF16)
            for k in range(KC):
                kb.dma(pool, Wo[:, k, :], IN("wout")[k * 128:(k + 1) * 128, :], [], [Wo_b])
            Wrf, Wrf_b = _sb(es, nc, "Wrf", [128, KC, 32], F32)
            Wr, Wr_b = _sb(es, nc, "Wr", [128, 2, KC, 32], BF16)
            kb.dma(sp, Wrf[:], IN("wr").rearrange("(k p) e -> p k e", p=128), [], [Wrf_b])
            kb.op(dve, [Wrf_b], [Wr_b], lambda: nc.vector.tensor_copy(out=Wr[:, 0, :, :], in_=Wrf[:]))
            kb.op(dve, [Wrf_b, Wr_b], [Wr_b],
                  lambda: nc.vector.tensor_tensor(out=Wr[:, 1, :, :], in0=Wrf[:], in1=Wr[:, 0, :, :], op=ALU.subtract))
            brb, brb_b = _sb(es, nc, "brb", [128, 32], F32)
            kb.dma(sp, brb[:], IN("brt").partition_broadcast(128), [], [brb_b])
            bc = {}
            for who, a in (("l", 0), ("c", 1)):
                if who == "c" and last:
                    continue
                for nm2, si in (("gt1", 2), ("sh2", 3), ("gs2", 4)):
                    bc[(who, nm2)] = load_bc(es, "bc_%s_%s" % (who, nm2), modv[a, si:si + 1, :])
            Ys = [_sb(es, nc, "Yin%d" % i, [128, 4, 512], BF16) for i in range(3)]
            hT, hT_b = _sb(es, nc, "mhT", [128, KC, 512], BF16)
            mT, mT_b = _sb(es, nc, "mT", [128, KC, 512], BF16)
            sigs = Pool8([_sb(es, nc, "msig%d" % i, [128, 512], F32) for i in range(2)])
            tmps = Pool8([_sb(es, nc, "mtmp%d" % i, [128, 512], F32) for i in range(2)])
            macc, macc_b = _sb(es, nc, "macc", [128, 512], F32)
            xts = Pool8([_sb(es, nc, "mxt%d" % i, [128, D], F32) for i in range(2)])
            xns = Pool8([_sb(es, nc, "mxn%d" % i, [128, D], F32) for i in range(2)])
            t2, t2_b = _sb(es, nc, "mt2", [128, D], F32)
            h2, h2_b = _sb(es, nc, "mh2", [128, D], F32)
            junk, junk_b = _sb(es, nc, "mjunk", [128, D], BF16)
            h2Tf, h2Tf_b = _sb(es, nc, "h2Tlo", [128, KC, 128], BF16)
            hbs = Pool8([_sb(es, nc, "mhb%d" % i, [128, 2, D], BF16) for i in range(2)])
            h2Tb, h2Tb_b = _sb(es, nc, "h2Tb", [128, KC, 512], BF16)
            st, st_b = _sb(es, nc, "mst", [128, 8], F32)
            rt, rt_b = _sb(es, nc, "mrt", [128, 4, 32], F32)
            top, top_b = _sb(es, nc, "mtop", [128, 16], F32)
            gws = Pool8([_sb(es, nc, "mgw%d" % i, [128, 32], F32) for i in range(2)])
            psf = Pool8([_ps(es, nc, "mps%d" % i, [128, 512], F32) for i in range(6)])
            psTa = _ps(es, nc, "mpsTa", [128, 1024], BF16)
            psTb = _ps(es, nc, "mpsTb", [128, 1024], BF16)
            YT = (YA_T, YB_T, YC_T)
            CUT = 99
            for (q0, n, ext0, is_ctx) in q_chunks(512):
                if CUT <= 0:
                    break
                who = "c" if is_ctx else "l"
                nt = n // 128
                for i in range(3):
                    kb.dma(sp, Ys[i][0][:, :, :n], YT[i].rearrange("(k p) t -> p k t", p=128)[:, :, q0:q0 + n], [kb.dbuf("Y%d" % i)], [Ys[i][1]])
                kb.dma(sp, hT[:, :, :n], hT_q.rearrange("(k p) t -> p k t", p=128)[:, :, q0:q0 + n], [kb.dbuf("hT_q")], [hT_b])
                for fc in range(KC):
                    for i in range(3):
                        pz, pz_b = psf.get()
                        pg, pg_b = psf.get()
                        for k in range(4):
                            kb.op(pe, [Wbr_b, Ys[i][1]], [pz_b],
                                  lambda: nc.tensor.matmul(pz[:, :n], lhsT=Wbr[:, i, k, fc * 128:(fc + 1) * 128], rhs=Ys[i][0][:, k, :n],
                                                           start=(k == 0), stop=(k == 3)))
                        for k in range(KC):
                            kb.op(pe, [Wg_b, hT_b], [pg_b],
                                  lambda: nc.tensor.matmul(pg[:, :n], lhsT=Wg[:, k, i * D + fc * 128:i * D + (fc + 1) * 128], rhs=hT[:, k, :n],
                                                           start=(k == 0), stop=(k == KC - 1)))
                        sg, sg_b = sigs.get()
                        kb.op(act, [pg_b], [sg_b], lambda: nc.scalar.activation(out=sg[:, :n], in_=pg[:, :n], func=AF.Sigmoid))
                        if i == 0:
                            kb.op(dve, [pz_b, sg_b], [macc_b],
                                  lambda: nc.vector.tensor_tensor(out=macc[:, :n], in0=pz[:, :n], in1=sg[:, :n], op=ALU.mult))
                        else:
                            tp, tp_b = tmps.get()
                            kb.op(dve, [pz_b, sg_b], [tp_b],
                                  lambda: nc.vector.tensor_tensor(out=tp[:, :n], in0=pz[:, :n], in1=sg[:, :n], op=ALU.mult))
                            if i == 1:
                                kb.op(pool, [tp_b, macc_b], [macc_b],
                                      lambda: nc.gpsimd.tensor_tensor(out=macc[:, :n], in0=macc[:, :n], in1=tp[:, :n], op=ALU.add))
                            else:
                                kb.op(pool, [tp_b, macc_b], [mT_b],
                                      lambda: nc.gpsimd.tensor_tensor(out=mT[:, fc, :n], in0=macc[:, :n], in1=tp[:, :n], op=ALU.add))
                if CUT <= 1:
                    continue
                gt1, gt1_b = bc[(who, "gt1")]
                gs2, gs2_b = bc[(who, "gs2")]
                sh2, sh2_b = bc[(who, "sh2")]
                for t in range(nt):
                    ts_ = slice(t * 128, (t + 1) * 128)
                    xt, xt_b = xts.get()
                    src = IN("ctx")[ext0 - T_ALL + t * 128: ext0 - T_ALL + (t + 1) * 128, :] if is_ctx else IN("x_ext")[ext0 + t * 128:ext0 + (t + 1) * 128, :]
                    kb.dma(sp, xt[:], src, [], [xt_b])
                    for nn in range(2):
                        py, py_b = psf.get()
                        for fc in range(KC):
                            kb.op(pe, [mT_b, Wo_b], [py_b],
                                  lambda: nc.tensor.matmul(py[:, :], lhsT=mT[:, fc, ts_], rhs=Wo[:, fc, nn * 512:(nn + 1) * 512],
                                                           start=(fc == 0), stop=(fc == KC - 1)))
                        kb.op(dve, [py_b, gt1_b], [t2_b],
                              lambda: nc.vector.tensor_tensor(out=t2[:, nn * 512:(nn + 1) * 512], in0=py[:, :], in1=gt1[:, nn * 512:(nn + 1) * 512], op=ALU.mult))
                    xn, xn_b = xns.get()
                    kb.op(pool, [t2_b, xt_b], [xn_b], lambda: nc.gpsimd.tensor_tensor(out=xn[:], in0=t2[:], in1=xt[:], op=ALU.add))
                    kb.dma(sp, x_res[q0 + t * 128:q0 + (t + 1) * 128, :], xn[:], [xn_b], [kb.dbuf("x_res")])
                    if CUT <= 2:
                        continue
                    rstd = rstd_of(xn[:], xn_b, D, junk[:], junk_b, st, st_b, 0)
                    kb.op(dve, [xn_b, st_b, gs2_b], [t2_b],
                          lambda: nc.vector.scalar_tensor_tensor(out=t2[:], in0=xn[:], scalar=rstd, in1=gs2[:], op0=ALU.mult, op1=ALU.mult))
                    kb.op(pool, [t2_b, sh2_b], [h2_b], lambda: nc.gpsimd.tensor_tensor(out=h2[:], in0=t2[:], in1=sh2[:], op=ALU.add))
                    hb, hb_b = hbs.get()
                    kb.op(act, [h2_b], [hb_b], lambda: nc.scalar.copy(out=hb[:, 0, :], in_=h2[:]))
                    kb.op(dve, [h2_b, hb_b], [hb_b],
                          lambda: nc.vector.tensor_tensor(out=hb[:, 1, :], in0=h2[:], in1=hb[:, 0, :], op=ALU.subtract))
                    for part, (pt_, pt_b) in enumerate((psTa, psTb)):
                        for k in range(KC):
                            kb.op(pe, [hb_b, ident_b_b], [pt_b],
                                  lambda: nc.tensor.transpose(pt_[:, k * 128:(k + 1) * 128], hb[:, part, k * 128:(k + 1) * 128], ident_b[:]))
                    kb.op(act, [psTa[1]], [h2Tb_b],
                          lambda: nc.scalar.copy(out=h2Tb[:, :, ts_], in_=psTa[0][:].rearrange("p (k t) -> p k t", k=KC)))
                    kb.op(dve, [psTb[1]], [h2Tf_b],
                          lambda: nc.vector.tensor_copy(out=h2Tf[:], in_=psTb[0][:].rearrange("p (k t) -> p k t", k=KC)))
                    if CUT <= 3:
                        continue
                    pl, pl_b = psf.get()
                    for k in range(KC):
                        kb.op(pe, [h2Tb_b, Wr_b], [pl_b],
                              lambda: nc.tensor.matmul(pl[:, 0:32], lhsT=h2Tb[:, k, ts_], rhs=Wr[:, 0, k, :], start=(k == 0), stop=False))
                        kb.op(pe, [h2Tb_b, Wr_b], [pl_b],
                              lambda: nc.tensor.matmul(pl[:, 0:32], lhsT=h2Tb[:, k, ts_], rhs=Wr[:, 1, k, :], start=False, stop=False))
                        kb.op(pe, [h2Tf_b, Wr_b], [pl_b],
                              lambda: nc.tensor.matmul(pl[:, 0:32], lhsT=h2Tf[:, k, :], rhs=Wr[:, 0, k, :], start=False, stop=(k == KC - 1)))
                    kb.op(dve, [pl_b, brb_b], [rt_b], lambda: nc.vector.tensor_tensor(out=rt[:, 0, :], in0=pl[:, 0:32], in1=brb[:], op=ALU.add))
                    kb.op(dve, [rt_b], [top_b], lambda: nc.vector.max(out=top[:, 0:8], in_=rt[:, 0, :]))
                    kb.op(dve, [rt_b, top_b], [rt_b],
                          lambda: nc.vector.tensor_scalar(out=rt[:, 1, :], in0=rt[:, 0, :], scalar1=top[:, 3:4], scalar2=None, op0=ALU.is_ge))
                    kb.op(dve, [top_b], [top_b],
                          lambda: nc.vector.tensor_scalar(out=top[:, 8:9], in0=top[:, 0:1], scalar1=-1.0, scalar2=None, op0=ALU.mult))
                    kb.op(act, [rt_b, top_b], [rt_b],
                          lambda: nc.scalar.activation(out=rt[:, 2, :], in_=rt[:, 0, :], func=AF.Exp, bias=top[:, 8:9], scale=1.0))
                    kb.op(dve, [rt_b], [rt_b], lambda: nc.vector.tensor_tensor(out=rt[:, 3, :], in0=rt[:, 2, :], in1=rt[:, 1, :], op=ALU.mult))
                    kb.op(dve, [rt_b], [top_b], lambda: nc.vector.reduce_sum(out=top[:, 9:10], in_=rt[:, 3, :], axis=AX.X))
                    kb.op(dve, [top_b], [top_b], lambda: nc.vector.reciprocal(out=top[:, 10:11], in_=top[:, 9:10]))
                    gw, gw_b = gws.get()
                    kb.op(dve, [rt_b, top_b], [gw_b],
                          lambda: nc.vector.tensor_scalar(out=gw[:], in0=rt[:, 3, :], scalar1=top[:, 10:11], scalar2=None, op0=ALU.mult))
                    kb.dma(sp, gw_d[q0 + t * 128:q0 + (t + 1) * 128, :], gw[:], [gw_b], [kb.dbuf("gw_d")])
                kb.dma(sp, h2T.rearrange("(k p) t -> p k t", p=128)[:, :, q0:q0 + n], h2Tb[:, :, :n], [h2Tb_b], [kb.dbuf("h2T")])

    def phase_moe():
        with ExitStack() as es:
            gus = Pool8([_sb(es, nc, "gu%d" % i, [128, KC, 2 * D], BF16) for i in range(2)])
            dns = Pool8([_sb(es, nc, "dn%d" % i, [128, KC, D], BF16) for i in range(2)])
            hT, hT_b = _sb(es, nc, "eh2T", [128, KC, 512], BF16)
            acc, acc_b = _sb(es, nc, "eacc", [128, 4, D], F32)
            aTs = Pool8([_sb(es, nc, "eaT%d" % i, [128, KC, 512], BF16) for i in range(2)])
            gg, gg_b = _sb(es, nc, "egg", [128, 512], F32)
            ss, ss_b = _sb(es, nc, "ess", [128, 512], F32)
            uu, uu_b = _sb(es, nc, "euu", [128, 512], F32)
            gs_, gs_b = _sb(es, nc, "egs", [128, 512], F32)
            gw, gw_b = _sb(es, nc, "egw", [128, 4, 32], F32)
            sel, sel_b = _sb(es, nc, "esel", [32, NE, 128], BF16)
            bdf, bdf_b = _sb(es, nc, "ebdf", [32, D], F32)
            bd, bd_b = _sb(es, nc, "ebd", [32, D], BF16)
            bg, bg_b = _sb(es, nc, "ebg", [128, NE, 16], F32)
            xts = Pool8([_sb(es, nc, "ext%d" % i, [128, D], F32) for i in range(2)])
            xns = Pool8([_sb(es, nc, "exn%d" % i, [128, D], F32) for i in range(2)])
            t2, t2_b = _sb(es, nc, "et2", [128, D], F32)
            junk, junk_b = _sb(es, nc, "ejunk", [128, D], BF16)
            st, st_b = _sb(es, nc, "est", [128, 8], F32)
            psf = Pool8([_ps(es, nc, "eps%d" % i, [128, 512], F32) for i in range(8)])
            kb.op(dve, [], [bdf_b], lambda: nc.vector.memset(bdf[:], 0.0))
            kb.dma(sp, bdf[0:NE, :], IN("bdn")[:, :], [], [bdf_b])
            kb.op(dve, [bdf_b], [bd_b], lambda: nc.vector.tensor_copy(out=bd[:], in_=bdf[:]))
            for e in range(NE):
                kb.op(dve, [ones_f_b, ident_f_b], [sel_b],
                      lambda: nc.vector.tensor_scalar(out=sel[:, e, :], in0=ones_f[0:32, :], scalar1=ident_f[0:32, e:e + 1], scalar2=None, op0=ALU.mult))
            kb.dma(sp, bg[:], IN("bgu")[:, :, :], [], [bg_b])
            gt2l, gt2l_b = load_bc(es, "gt2l", modv[0, 5:6, :])
            if not last:
                gt2c, gt2c_b = load_bc(es, "gt2c", modv[1, 5:6, :])
            else:
                gfin, gfin_b = _sb(es, nc, "gfin", [128, D], F32)
                kb.dma(sp, gfin[:], IN("gvecs")[2:3, :].partition_broadcast(128), [], [gfin_b])
            for (q0, n, ext0, is_ctx) in q_chunks(512):
                nt = n // 128
                kb.dma(sp, hT[:, :, :n], h2T.rearrange("(k p) t -> p k t", p=128)[:, :, q0:q0 + n], [kb.dbuf("h2T")], [hT_b])
                kb.dma(sp, gw[:, :nt, :], gw_d[q0:q0 + n, :].rearrange("(t p) e -> p t e", p=128), [kb.dbuf("gw_d")], [gw_b])
                kb.op(dve, [], [acc_b], lambda: nc.vector.memset(acc[:], 0.0))
                for e in range(NE):
                    gu, gu_b = gus.get()
                    dn, dn_b = dns.get()
                    for k in range(KC):
                        kb.dma(pool, gu[:, k, :], IN("wgu")[e, k * 128:(k + 1) * 128, :], [], [gu_b])
                    for k in range(KC):
                        kb.dma(pool, dn[:, k, :], IN("wdn")[e, k * 128:(k + 1) * 128, :], [], [dn_b])
                    aT, aT_b = aTs.get()
                    for fc in range(KC):
                        pg, pg_b = psf.get()
                        pu, pu_b = psf.get()
                        for k in range(KC):
                            kb.op(pe, [gu_b, hT_b], [pg_b],
                                  lambda: nc.tensor.matmul(pg[:, :n], lhsT=gu[:, k, fc * 128:(fc + 1) * 128], rhs=hT[:, k, :n], start=(k == 0), stop=(k == KC - 1)))
                        for k in range(KC):
                            kb.op(pe, [gu_b, hT_b], [pu_b],
                                  lambda: nc.tensor.matmul(pu[:, :n], lhsT=gu[:, k, D + fc * 128:D + (fc + 1) * 128], rhs=hT[:, k, :n], start=(k == 0), stop=(k == KC - 1)))
                        kb.op(dve, [pg_b, bg_b], [gg_b],
                              lambda: nc.vector.tensor_scalar(out=gg[:, :n], in0=pg[:, :n], scalar1=bg[:, e, fc:fc + 1], scalar2=LIM, op0=ALU.add, op1=ALU.min))
                        kb.op(act, [gg_b], [ss_b], lambda: nc.scalar.activation(out=ss[:, :n], in_=gg[:, :n], func=AF.Sigmoid, scale=ALPHA))
                        kb.op(dve, [pu_b, bg_b], [uu_b],
                              lambda: nc.vector.tensor_scalar(out=uu[:, :n], in0=pu[:, :n], scalar1=bg[:, e, 8 + fc:9 + fc], scalar2=LIM, op0=ALU.add, op1=ALU.min))
                        kb.op(dve, [uu_b], [uu_b],
                              lambda: nc.vector.tensor_scalar(out=uu[:, :n], in0=uu[:, :n], scalar1=-LIM, scalar2=1.0, op0=ALU.max, op1=ALU.add))
                        kb.op(pool, [gg_b, ss_b], [gs_b], lambda: nc.gpsimd.tensor_tensor(out=gs_[:, :n], in0=gg[:, :n], in1=ss[:, :n], op=ALU.mult))
                        kb.op(dve, [uu_b, gs_b], [aT_b], lambda: nc.vector.tensor_tensor(out=aT[:, fc, :n], in0=uu[:, :n], in1=gs_[:, :n], op=ALU.mult))
                    for t in range(nt):
                        for nn in range(2):
                            py, py_b = psf.get()
                            kb.op(pe, [sel_b, bd_b], [py_b],
                                  lambda: nc.tensor.matmul(py[:, :], lhsT=sel[:, e, :], rhs=bd[:, nn * 512:(nn + 1) * 512], start=True, stop=False))
                            for fc in range(KC):
                                kb.op(pe, [aT_b, dn_b], [py_b],
                                      lambda: nc.tensor.matmul(py[:, :], lhsT=aT[:, fc, t * 128:(t + 1) * 128], rhs=dn[:, fc, nn * 512:(nn + 1) * 512],
                                                               start=False, stop=(fc == KC - 1)))
                            kb.op(dve, [py_b, gw_b, acc_b], [acc_b],
                                  lambda: nc.vector.scalar_tensor_tensor(out=acc[:, t, nn * 512:(nn + 1) * 512], in0=py[:, :], scalar=gw[:, t, e:e + 1],
                                                                         in1=acc[:, t, nn * 512:(nn + 1) * 512], op0=ALU.mult, op1=ALU.add))
                gt2, gt2_b = (gt2c, gt2c_b) if is_ctx else (gt2l, gt2l_b)
                for t in range(nt):
                    xt, xt_b = xts.get()
                    kb.dma(sp, xt[:], x_res[q0 + t * 128:q0 + (t + 1) * 128, :], [kb.dbuf("x_res")], [xt_b])
                    kb.op(dve, [acc_b, gt2_b], [t2_b], lambda: nc.vector.tensor_tensor(out=t2[:], in0=acc[:, t, :], in1=gt2[:], op=ALU.mult))
                    xn, xn_b = xns.get()
                    kb.op(pool, [t2_b, xt_b], [xn_b], lambda: nc.gpsimd.tensor_tensor(out=xn[:], in0=t2[:], in1=xt[:], op=ALU.add))
                    if last:
                        rstd = rstd_of(xn[:], xn_b, D, junk[:], junk_b, st, st_b, 0)
                        kb.op(dve, [xn_b, st_b, gfin_b], [t2_b],
                              lambda: nc.vector.scalar_tensor_tensor(out=t2[:], in0=xn[:], scalar=rstd, in1=gfin[:], op0=ALU.mult, op1=ALU.mult))
                        kb.dma(sp, out_x[q0 + t * 128:q0 + (t + 1) * 128, :], t2[:], [t2_b], [], is_output=True)
                    else:
                        kb.dma(sp, out_x[q0 + t * 128:q0 + (t + 1) * 128, :], xn[:], [xn_b], [], is_output=True)

    phases = [phase0, phase1, lambda: phase_attn("A"), lambda: phase_attn("C"), phase_attn_B, phase_merge, phase_moe]
    for i, ph in enumerate(phases):
        if i <= upto:
            ph()
            kb.barrier()
    kb.finish()
    es0.close()
    return nc


def _partner(nf):
    d = np.arange(4 * nf)
    return np.where((d // nf) % 2 == 0, d + nf, d - nf)


def _rope_full(pos, rot_dim):
    nf = rot_dim // 4
    rows = (pos // GRID_W).astype(np.float32)
    cols = (pos % GRID_W).astype(np.float32)
    inv = (np.float32(10000.0) ** (-np.arange(nf, dtype=np.float32) / np.float32(nf))).astype(np.float32)
    ar = (rows[:, None] * inv).astype(np.float32)
    ac = (cols[:, None] * inv).astype(np.float32)
    cos = np.concatenate([np.cos(ar), np.cos(ar), np.cos(ac), np.cos(ac)], axis=1).astype(np.float32)
    sin = np.concatenate([-np.sin(ar), np.sin(ar), -np.sin(ac), np.sin(ac)], axis=1).astype(np.float32)
    return cos.T.copy(), sin.T.copy()


def host_tables(cfg, j):
    T_OWN, T_ALL, L, T_EXT = cfg.T_OWN, cfg.T_ALL, cfg.L, cfg.T_EXT
    pos = np.concatenate([np.arange(T_OWN) + j * T_OWN, np.arange(T_OWN) + (1 - j) * T_OWN]).astype(np.int64)
    ca, sa = _rope_full(pos, 64)
    cc, sc = _rope_full(pos, 32)
    cosA = np.ones((128, T_EXT), np.float32)
    sinA = np.zeros((128, T_EXT), np.float32)
    cosA[0:64, :T_ALL] = ca
    cosA[64:128, :T_ALL] = ca
    sinA[0:64, :T_ALL] = sa
    sinA[64:128, :T_ALL] = sa
    cosC = np.ones((96, T_EXT), np.float32)
    sinC = np.zeros((96, T_EXT), np.float32)
    cosC[0:32, :T_ALL] = cc
    sinC[0:32, :T_ALL] = sc
    return {"cosA": cosA, "sinA": sinA, "cosC": cosC, "sinC": sinC}


def host_bias_consts(cfg, j):
    c = np.arange(GRID_W)
    col_start = np.clip(c - 8, 0, GRID_W - 16)
    colmask = (c[None, :] >= col_start[:, None]) & (c[None, :] < col_start[:, None] + 16)
    bc = np.zeros((33, GRID_W, GRID_W), np.float32)
    for r in range(31):
        kc_, qc_ = np.meshgrid(c, c, indexing="ij")
        bc[r] = 8.0 * ((kc_ - qc_ + 15) == r) * colmask.T
    bc[31] = NEG * (1.0 - colmask.T)
    bc[32] = NEG * colmask.T
    def valid(kind, b, dr):
        if kind == "interior":
            return -4 <= dr <= 3
        if kind == "first":
            return 0 <= b + dr <= 7
        if kind == "last":
            return -4 <= b + dr <= 3
    kinds = [("first" if j == 0 else "interior"), "interior", ("last" if j == 1 else "interior")]
    sel = np.zeros((3, 4, 33, 15), np.float32)
    for ci, kind in enumerate(kinds):
        for b in range(4):
            for ip in range(15):
                dr = 7 - ip
                if valid(kind, b, dr):
                    sel[ci, b, 0:32, ip] = 1.0
                else:
                    sel[ci, b, 31, ip] = 1.0
                    sel[ci, b, 32, ip] = 1.0
    return {"bconst": bc.reshape(33, 4096), "bsel": sel}


def host_layer(cfg, inp, l):
    f = lambda a: np.ascontiguousarray(a, dtype=np.float32)
    w_in = inp["w_in"][l]
    qa, ka, va = w_in[:, 0:512], w_in[:, 512:1024], w_in[:, 1024:1536]
    qb, kb_, vb = w_in[:, 1536:2048], w_in[:, 2048:2560], w_in[:, 2560:3072]
    cq, ckv, ckr = w_in[:, 3072:3456], w_in[:, 3456:3712], w_in[:, 3712:3744]
    gates = w_in[:, 3744:6816]
    pA = _partner(16)
    idxA = (np.arange(512) // 64) * 64
    swA = idxA + pA[np.arange(512) % 64]
    pC = _partner(8)
    w1 = np.concatenate([qa, qa[:, swA], ka, ka[:, swA], qb, kb_, ckr, ckr[:, pC], va, vb, cq, ckv], axis=1)
    wq = inp["w_q_b"][l]
    main = np.zeros((C_QR, 768), np.float32)
    sw = np.zeros((C_QR, 768), np.float32)
    for h in range(C_HEADS):
        pe_ = wq[:, h * 96 + 64:h * 96 + 96]
        main[:, h * 96:h * 96 + 32] = pe_
        main[:, h * 96 + 32:h * 96 + 96] = wq[:, h * 96:h * 96 + 64]
        sw[:, h * 96:h * 96 + 32] = pe_[:, pC]
    wkv = inp["w_kv_b"][l].reshape(C_KVR, C_HEADS, 128)
    wkvb = np.concatenate([wkv[:, :, :64].reshape(C_KVR, 512), wkv[:, :, 64:].reshape(C_KVR, 512)], axis=1)
    rpb = inp["rpb"][l]
    rpbT = np.zeros((B_HEADS, 33, 15), np.float32)
    rpbT[:, 0:31, :] = np.transpose(rpb[:, ::-1, :], (0, 2, 1))
    rpbT[:, 31, :] = 1.0
    rpbT[:, 32, :] = 1.0
    wgu = inp["w_gate_up"][l]
    NE = cfg.NE
    wgu2 = np.concatenate([wgu[:NE, :, 0::2], wgu[:NE, :, 1::2]], axis=2)
    bgu = inp["b_gate_up"][l][:NE]
    bg = bgu[:, 0::2].reshape(NE, 8, 128)
    bu = bgu[:, 1::2].reshape(NE, 8, 128)
    bgu2 = np.concatenate([np.transpose(bg, (2, 0, 1)), np.transpose(bu, (2, 0, 1))], axis=2)
    d = {
        "w_mod": f(inp["w_mod"][l]), "b_mod": f(inp["b_mod"][l][None, :]),
        "gvecs": f(np.stack([inp["g_mix"][l], inp["g_ffn"][l], inp["g_final"]])),
        "w1": f(w1), "wg": f(gates), "ident": np.eye(128, dtype=np.float32),
        "lam": f(np.stack([inp["lam_q1"][l], inp["lam_k1"][l], inp["lam_q2"][l], inp["lam_k2"][l]])),
        "gsub": f(inp["g_subln"][l].reshape(128, 1)),
        "gqa": f(inp["g_q_a"][l].reshape(3, 128).T), "gkva": f(inp["g_kv_a"][l].reshape(2, 128).T),
        "wqb": f(np.concatenate([main, sw], axis=1)), "wkvb": f(wkvb), "rpbT": f(rpbT),
        "wbr": f(np.stack([inp["w_br_a"][l], inp["w_br_b"][l], inp["w_br_c"][l]])),
        "wout": f(inp["w_out"][l]), "wr": f(inp["w_router"][l][:, :NE] if NE == 32 else np.pad(inp["w_router"][l][:, :NE], ((0, 0), (0, 32 - NE)))),
        "brt": f((inp["b_router"][l][:NE] if NE == 32 else np.pad(inp["b_router"][l][:NE], (0, 32 - NE), constant_values=-1e4))[None, :]),
        "wgu": f(wgu2), "bgu": f(bgu2), "wdn": f(inp["w_down"][l][:NE]), "bdn": f(inp["b_down"][l][:NE]),
    }
    return d


def host_core(cfg, x_batch, ctx_b, c_b, c_ctx, j):
    T_OWN = cfg.T_OWN
    own = x_batch[j * T_OWN:(j + 1) * T_OWN]
    oth = x_batch[(1 - j) * T_OWN:(2 - j) * T_OWN]
    cvec = np.stack([c_b.reshape(KC, 128).T, c_ctx.reshape(KC, 128).T], axis=2)
    d = {"x_ext": np.ascontiguousarray(np.concatenate([own, oth], axis=0), dtype=np.float32),
         "ctx": np.ascontiguousarray(ctx_b, dtype=np.float32),
         "cvec": np.ascontiguousarray(cvec, dtype=np.float32)}
    d.update(host_tables(cfg, j))
    d.update(host_bias_consts(cfg, j))
    return d


N_CORES = 8


def _run_layer(cfg, inp, layer, last, x_full, ctx_full):
    nc = build_program(cfg, layer, last)
    lay = host_layer(cfg, inp, layer)
    maps = []
    for core in range(N_CORES):
        b, j = core // 2, core % 2
        d = dict(lay)
        d.update(host_core(cfg, x_full[b], ctx_full[b], inp["c"][b], inp["c_ctx"], j))
        maps.append(d)
    res = run_bass_kernel_spmd(nc, maps, core_ids=list(range(N_CORES)))
    return [np.asarray(r["out_x"]) for r in res.results]


def kernel(**inputs):
    inp = {k: np.asarray(v) for k, v in inputs.items()}
    B, SEQ, _ = inp["x"].shape
    L = inp["ctx"].shape[1]
    cfg = Cfg(t_own=SEQ // 2, ctx=L, n_exp=inp["w_router"].shape[2], depth=inp["w_mod"].shape[0])
    T_OWN = cfg.T_OWN
    x = inp["x"].astype(np.float32)
    xc = inp["ctx"].astype(np.float32)
    depth = cfg.DEPTH
    for layer in range(depth):
        last = layer == depth - 1
        outs = _run_layer(cfg, inp, layer, last, x, xc)
        x_new = np.empty_like(x)
        for core in range(N_CORES):
            b, j = core // 2, core % 2
            x_new[b, j * T_OWN:(j + 1) * T_OWN] = outs[core][:T_OWN]
        if not last:
            xc = np.stack([outs[2 * b][T_OWN:] for b in range(B)], axis=0)
        x = x_new
    return x
```

```python
import math
from contextlib import ExitStack
import numpy as np
import concourse.bass as bass
import concourse.mybir as mybir
from concourse.bass_utils import run_bass_kernel_spmd

F32 = mybir.dt.float32
BF16 = mybir.dt.bfloat16
AF = mybir.ActivationFunctionType
ALU = mybir.AluOpType
AX = mybir.AxisListType

D = 1024
KC = 8
GRID_W = 64
A_HEADS, A_HD = 4, 64
B_HEADS = 8
C_HEADS, C_NOPE, C_ROPE, C_V = 8, 64, 32, 64
C_QR, C_KVR = 384, 256
EPS = 1e-6
NEG = -30000.0
LIM = 7.0
ALPHA = 1.702
FM_COLS = 3136
TM_COLS = 1664
W1_COLS = FM_COLS + TM_COLS


class Cfg:
    def __init__(self, t_own=4096, ctx=256, n_exp=32, depth=2):
        self.T_OWN = t_own
        self.T_ALL = 2 * t_own
        self.L = ctx
        self.T_EXT = self.T_ALL + ctx
        self.NE = n_exp
        self.DEPTH = depth
        self.ROWS_CORE = t_own // GRID_W
        self.ROWS_ALL = 2 * self.ROWS_CORE


class Sem:
    def __init__(self, h):
        self.h = h
        self.count = 0


class Buf:
    __slots__ = ("name", "w", "r")

    def __init__(self, name=""):
        self.name = name
        self.w = None
        self.r = {}


class Eng:
    def __init__(self, kb, name, e):
        self.name = name
        self.e = e
        self.sem = kb.new_sem("e_" + name)
        self.waited = {}


class KB:
    def __init__(self, nc):
        self.nc = nc
        self.nsem = 0
        self.pe = Eng(self, "pe", nc.tensor)
        self.act = Eng(self, "act", nc.scalar)
        self.dve = Eng(self, "dve", nc.vector)
        self.pool = Eng(self, "pool", nc.gpsimd)
        self.sp = Eng(self, "sp", nc.sync)
        self.dpool = {}
        for q in (self.sp, self.pool, self.act):
            self.dpool[q.name] = [self.new_sem("d_%s%d" % (q.name, i)) for i in range(24)]
        self.drr = {"sp": 0, "pool": 0, "act": 0}
        self.dram_bufs = {}
        self.out_events = []

    def new_sem(self, name):
        self.nsem += 1
        return Sem(self.nc.semaphore(name).__enter__())

    def dbuf(self, key):
        b = self.dram_bufs.get(key)
        if b is None:
            b = Buf(str(key))
            self.dram_bufs[key] = b
        return b

    def _wait(self, E, reads, writes):
        deps = {}
        for b in reads:
            if b.w is not None:
                s, v = b.w
                if deps.get(s, 0) < v:
                    deps[s] = v
        for b in writes:
            if b.w is not None:
                s, v = b.w
                if deps.get(s, 0) < v:
                    deps[s] = v
            for s, v in b.r.items():
                if deps.get(s, 0) < v:
                    deps[s] = v
        for s, v in deps.items():
            if s is E.sem and E is self.pe:
                continue
            if E.waited.get(s, 0) < v:
                E.e.wait_ge(s.h, v)
                E.waited[s] = v

    def op(self, E, reads, writes, fn):
        self._wait(E, reads, writes)
        ins = fn()
        E.sem.count += 1
        ins.then_inc(E.sem.h, 1)
        v = E.sem.count
        for b in reads:
            b.r[E.sem] = v
        for b in writes:
            b.w = (E.sem, v)
            b.r = {}
        return ins

    def dma(self, Q, out, in_, reads, writes, is_output=False):
        self._wait(Q, reads, writes)
        pool = self.dpool[Q.name]
        i = self.drr[Q.name]
        self.drr[Q.name] = (i + 1) % len(pool)
        ds = pool[i]
        if ds.count > 0 and Q.waited.get(ds, 0) < ds.count:
            Q.e.wait_ge(ds.h, ds.count)
            Q.waited[ds] = ds.count
        Q.e.dma_start(out=out, in_=in_).then_inc(ds.h, 16)
        ds.count += 16
        v = ds.count
        for b in reads:
            b.r[ds] = v
        for b in writes:
            b.w = (ds, v)
            b.r = {}
        if is_output:
            self.out_events.append((ds, v))

    def barrier(self):
        sems = [e.sem for e in (self.pe, self.act, self.dve, self.pool, self.sp)]
        for p in self.dpool.values():
            sems.extend(p)
        for E in (self.pe, self.act, self.dve, self.pool, self.sp):
            for s in sems:
                if s.count > 0 and E.waited.get(s, 0) < s.count and not (s is E.sem):
                    E.e.wait_ge(s.h, s.count)
                    E.waited[s] = s.count

    def finish(self):
        for b in self.dram_bufs.values():
            if b.w is not None:
                self.out_events.append(b.w)
        for s, v in self.out_events:
            if self.sp.waited.get(s, 0) < v:
                self.sp.e.wait_ge(s.h, v)
                self.sp.waited[s] = v


class Pool8:
    def __init__(self, items):
        self.items = items
        self.i = 0

    def get(self):
        it = self.items[self.i]
        self.i = (self.i + 1) % len(self.items)
        return it


def _sb(es, nc, name, shape, dt):
    t = es.enter_context(nc.sbuf_tensor(name, shape, dt))
    return t, Buf(name)


def _ps(es, nc, name, shape, dt):
    t = es.enter_context(nc.psum_tensor(name, shape, dt))
    return t, Buf(name)


def build_program(cfg, layer, last, upto=99, dump=()):
    nc = bass.Bass("TRN2", target_bir_lowering=False)
    kb = KB(nc)
    T_OWN, T_ALL, L, T_EXT, NE = cfg.T_OWN, cfg.T_ALL, cfg.L, cfg.T_EXT, cfg.NE
    T_Q = T_OWN + (0 if last else L)
    lam_init = 0.8 - 0.6 * math.exp(-0.3 * layer)

    def din(name, shape, dt=F32):
        return nc.dram_tensor(name, list(shape), dt, kind="ExternalInput").ap()

    def dscr(name, shape, dt):
        kind = "ExternalOutput" if name in dump else None
        if kind:
            return nc.dram_tensor(name, list(shape), dt, kind=kind).ap()
        return nc.dram_tensor(name, list(shape), dt).ap()

    IN_SHAPES = {
        "x_ext": [T_ALL, D], "ctx": [L, D], "cvec": [128, KC, 2], "w_mod": [D, 6 * D], "b_mod": [1, 6 * D],
        "gvecs": [3, D], "w1": [D, W1_COLS], "wg": [D, 3 * D], "ident": [128, 128],
        "cosA": [128, T_EXT], "sinA": [128, T_EXT], "cosC": [96, T_EXT], "sinC": [96, T_EXT],
        "lam": [4, 64], "gsub": [128, 1], "gqa": [128, 3], "gkva": [128, 2],
        "wqb": [C_QR, 2 * 768], "wkvb": [C_KVR, 1024],
        "rpbT": [B_HEADS, 33, 15], "bsel": [3, 4, 33, 15], "bconst": [33, 4096],
        "wbr": [3, 512, D], "wout": [D, D], "wr": [D, 32], "brt": [1, 32],
        "wgu": [NE, D, 2 * D], "bgu": [128, NE, 16], "wdn": [NE, D, D], "bdn": [NE, D],
    }
    _ins = {}

    def IN(name):
        if name not in _ins:
            _ins[name] = din(name, IN_SHAPES[name])
        return _ins[name]

    if last:
        out_x = nc.dram_tensor("out_x", [T_OWN, D], F32, kind="ExternalOutput").ap()
    else:
        out_x = nc.dram_tensor("out_x", [T_Q, D], F32, kind="ExternalOutput").ap()

    modv = dscr("modv", [2, 6, D], F32)
    hT_q = dscr("hT_q", [D, T_Q], BF16)
    QA_T = dscr("QA_T", [512, T_Q], BF16)
    KA_T = dscr("KA_T", [512, T_EXT], BF16)
    VA = dscr("VA", [T_EXT, 512], BF16)
    QB_T = dscr("QB_T", [512, T_Q], BF16)
    KB_T = dscr("KB_T", [512, T_EXT], BF16)
    VB = dscr("VB", [T_EXT, 512], BF16)
    QC_T = dscr("QC_T", [768, T_Q], BF16)
    KC_T = dscr("KC_T", [768, T_EXT], BF16)
    VC = dscr("VC", [T_EXT, 512], BF16)
    YA_T = dscr("YA_T", [512, T_Q], BF16)
    YB_T = dscr("YB_T", [512, T_Q], BF16)
    YC_T = dscr("YC_T", [512, T_Q], BF16)
    x_res = dscr("x_res", [T_Q, D], F32)
    h2T = dscr("h2T", [D, T_Q], BF16)
    gw_d = dscr("gw_d", [T_Q, 32], F32)
    btab = dscr("btab", [B_HEADS, 3, 4, 16, 4096], BF16)

    pe, act, dve, pool, sp = kb.pe, kb.act, kb.dve, kb.pool, kb.sp

    es0 = ExitStack()
    ident_f, ident_f_b = _sb(es0, nc, "ident_f", [128, 128], F32)
    ident_b, ident_b_b = _sb(es0, nc, "ident_b", [128, 128], BF16)
    ones_b, ones_b_b = _sb(es0, nc, "ones_b", [128, 128], BF16)
    ones_f, ones_f_b = _sb(es0, nc, "ones_f", [128, 128], F32)
    epsc, epsc_b = _sb(es0, nc, "epsc", [128, 1], F32)
    kb.dma(sp, ident_f[:], IN("ident")[:, :], [], [ident_f_b])
    kb.dma(pool, ident_b[:], IN("ident")[:, :], [], [ident_b_b])
    kb.op(dve, [], [ones_b_b], lambda: nc.vector.memset(ones_b[:], 1.0))
    kb.op(dve, [], [ones_f_b], lambda: nc.vector.memset(ones_f[:], 1.0))
    kb.op(dve, [], [epsc_b], lambda: nc.vector.memset(epsc[:], EPS))

    def q_chunks(width):
        res = []
        for c0 in range(0, T_OWN, width):
            res.append((c0, min(width, T_OWN - c0), c0, False))
        if not last:
            for c0 in range(0, L, width):
                res.append((T_OWN + c0, min(width, L - c0), T_ALL + c0, True))
        return res

    def phase0():
        with ExitStack() as es:
            cv, cv_b = _sb(es, nc, "cv", [128, KC, 2], F32)
            sc_, sc_b = _sb(es, nc, "silc", [128, KC, 2], F32)
            mod2, mod2_b = _sb(es, nc, "mod2", [2, 6 * D], F32)
            bm2, bm2_b = _sb(es, nc, "bm2", [2, 6 * D], F32)
            g2, g2_b = _sb(es, nc, "g2", [2, 2, D], F32)
            wts = [_sb(es, nc, "wm%d" % i, [128, KC, 512], F32) for i in range(2)]
            pss = [_ps(es, nc, "p0ps%d" % i, [128, 512], F32) for i in range(2)]
            kb.dma(sp, cv[:], IN("cvec")[:, :, :], [], [cv_b])
            kb.dma(sp, bm2[:], IN("b_mod").partition_broadcast(2), [], [bm2_b])
            for i in range(2):
                kb.dma(sp, g2[:, i, :], IN("gvecs")[i:i + 1, :].partition_broadcast(2), [], [g2_b])
            kb.op(act, [cv_b], [sc_b], lambda: nc.scalar.activation(out=sc_[:], in_=cv[:], func=AF.Silu))
            for nb in range(12):
                wt, wt_b = wts[nb % 2]
                ps, ps_b = pss[nb % 2]
                kb.dma(sp, wt[:], IN("w_mod")[:, nb * 512:(nb + 1) * 512].rearrange("(k p) n -> p k n", p=128), [], [wt_b])
                for k in range(KC):
                    kb.op(pe, [sc_b, wt_b], [ps_b],
                          lambda k=k: nc.tensor.matmul(ps[0:2, :], lhsT=sc_[:, k, :], rhs=wt[:, k, :],
                                                       start=(k == 0), stop=(k == KC - 1)))
                kb.op(dve, [ps_b, bm2_b], [mod2_b],
                      lambda: nc.vector.tensor_tensor(out=mod2[:, nb * 512:(nb + 1) * 512], in0=ps[0:2, :],
                                                      in1=bm2[:, nb * 512:(nb + 1) * 512], op=ALU.add))
            for (ci, gi) in ((1, 0), (4, 1)):
                kb.op(dve, [mod2_b, g2_b], [mod2_b],
                      lambda ci=ci, gi=gi: nc.vector.scalar_tensor_tensor(
                          out=mod2[:, ci * D:(ci + 1) * D], in0=mod2[:, ci * D:(ci + 1) * D], scalar=1.0,
                          in1=g2[:, gi, :], op0=ALU.add, op1=ALU.mult))
            kb.dma(sp, modv.rearrange("a s d -> a (s d)"), mod2[:], [mod2_b], [kb.dbuf("modv")])

    def load_bc(es, name, row_ap):
        n = row_ap.shape[-1]
        t, b = _sb(es, nc, name, [128, n], F32)
        kb.dma(sp, t[:], row_ap.partition_broadcast(128), [kb.dbuf("modv")], [b])
        return t, b

    def rstd_of(x_ap, xb, n, junk, junk_b, st, st_b, col):
        kb.op(act, [xb], [junk_b, st_b],
              lambda: nc.scalar.activation(out=junk, in_=x_ap, func=AF.Square, accum_out=st[:, col:col + 1]))
        kb.op(act, [st_b], [st_b],
              lambda: nc.scalar.activation(out=st[:, col + 1:col + 2], in_=st[:, col:col + 1], func=AF.Sqrt,
                                           scale=1.0 / n, bias=epsc[:, 0:1]))
        kb.op(dve, [st_b], [st_b],
              lambda: nc.vector.reciprocal(out=st[:, col + 2:col + 3], in_=st[:, col + 1:col + 2]))
        return st[:, col + 2:col + 3]

    def phase1():
        with ExitStack() as es:
            W1, W1_b = _sb(es, nc, "W1", [128, KC, W1_COLS], BF16)
            for k in range(KC):
                for c0 in range(0, W1_COLS, 1600):
                    kb.dma(pool, W1[:, k, c0:c0 + 1600], IN("w1")[k * 128:(k + 1) * 128, c0:c0 + 1600], [], [W1_b])
            Wqb, Wqb_b = _sb(es, nc, "Wqb", [128, 3, 1536], BF16)
            Wqf, Wqf_b = _sb(es, nc, "Wqf", [128, 1536], F32)
            gq, gq_b = _sb(es, nc, "gq", [128, 3], F32)
            gk, gk_b = _sb(es, nc, "gk", [128, 2], F32)
            Wkv, Wkv_b = _sb(es, nc, "Wkv", [128, 2, 1024], BF16)
            kb.dma(sp, gq[:], IN("gqa")[:, :], [], [gq_b])
            kb.dma(sp, gk[:], IN("gkva")[:, :], [], [gk_b])
            for k in range(3):
                kb.dma(sp, Wqf[:, :], IN("wqb")[k * 128:(k + 1) * 128, :], [], [Wqf_b])
                kb.op(dve, [Wqf_b, gq_b], [Wqb_b],
                      lambda k=k: nc.vector.tensor_scalar(out=Wqb[:, k, :], in0=Wqf[:, :], scalar1=gq[:, k:k + 1],
                                                          scalar2=None, op0=ALU.mult))
            for k in range(2):
                kb.dma(sp, Wqf[:, 0:1024], IN("wkvb")[k * 128:(k + 1) * 128, :], [], [Wqf_b])
                kb.op(dve, [Wqf_b, gk_b], [Wkv_b],
                      lambda k=k: nc.vector.tensor_scalar(out=Wkv[:, k, :], in0=Wqf[:, 0:1024], scalar1=gk[:, k:k + 1],
                                                          scalar2=None, op0=ALU.mult))
            gs_l, gs_l_b = load_bc(es, "gs1l", modv[0, 1:2, :])
            sh_l, sh_l_b = load_bc(es, "sh1l", modv[0, 0:1, :])
            gs_c, gs_c_b = load_bc(es, "gs1c", modv[1, 1:2, :])
            sh_c, sh_c_b = load_bc(es, "sh1c", modv[1, 0:1, :])

            xts = Pool8([_sb(es, nc, "xt%d" % i, [128, D], F32) for i in range(2)])
            tmp, tmp_b = _sb(es, nc, "p1tmp", [128, D], F32)
            junk, junk_b = _sb(es, nc, "p1junk", [128, D], BF16)
            hbs = Pool8([_sb(es, nc, "hb%d" % i, [128, D], BF16) for i in range(2)])
            st, st_b = _sb(es, nc, "p1st", [128, 16], F32)
            hTs = Pool8([_sb(es, nc, "hT%d" % i, [128, KC, 512], BF16) for i in range(2)])
            cst = Pool8([_sb(es, nc, "cst%d" % i, [128, 4, 512], F32) for i in range(1)])
            fm_st = Pool8([_sb(es, nc, "fmst%d" % i, [128, 4, 512], BF16) for i in range(2)])
            tm_st = Pool8([_sb(es, nc, "tmst%d" % i, [128, 512], BF16) for i in range(3)])
            r1, r1_b = _sb(es, nc, "r1", [128, 512], F32)
            r2, r2_b = _sb(es, nc, "r2", [128, 512], F32)
            cqn = Pool8([_sb(es, nc, "cqn%d" % i, [128, 384], BF16) for i in range(2)])
            cqnT, cqnT_b = _sb(es, nc, "cqnT", [128, 3, 512], BF16)
            ckvnT, ckvnT_b = _sb(es, nc, "ckvnT", [128, 2, 512], BF16)
            kpe, kpe_b = _sb(es, nc, "kpe", [32, 512], BF16)
            qc_st = Pool8([_sb(es, nc, "qcst%d" % i, [96, 512], BF16) for i in range(2)])
            psT, psT_b = _ps(es, nc, "p1psT", [128, 1024], BF16)
            psf = Pool8([_ps(es, nc, "p1ps%d" % i, [128, 512], F32) for i in range(7)])

            chunks = [(c0, 512, False) for c0 in range(0, T_ALL, 512)] + [(T_ALL + c0, min(512, L - c0), True) for c0 in range(0, L, 512)]
            for (e0, n, is_ctx) in chunks:
                is_q = (e0 < T_OWN) or (is_ctx and not last)
                q0 = e0 if e0 < T_OWN else (T_OWN + e0 - T_ALL)
                nt = n // 128
                gs, gs_b = (gs_c, gs_c_b) if is_ctx else (gs_l, gs_l_b)
                sh, sh_b = (sh_c, sh_c_b) if is_ctx else (sh_l, sh_l_b)
                hT, hT_b = hTs.get()
                cs, cs_b = cst.get()
                kb.dma(sp, cs[:, 0, :n], IN("cosA")[:, e0:e0 + n], [], [cs_b])
                kb.dma(sp, cs[:, 1, :n], IN("sinA")[:, e0:e0 + n], [], [cs_b])
                kb.dma(sp, cs[0:96, 2, :n], IN("cosC")[:, e0:e0 + n], [], [cs_b])
                kb.dma(sp, cs[0:96, 3, :n], IN("sinC")[:, e0:e0 + n], [], [cs_b])
                for t in range(nt):
                    xt, xt_b = xts.get()
                    src = IN("ctx")[e0 - T_ALL + t * 128: e0 - T_ALL + (t + 1) * 128, :] if is_ctx else IN("x_ext")[e0 + t * 128:e0 + (t + 1) * 128, :]
                    kb.dma(sp, xt[:], src, [], [xt_b])
                    rstd = rstd_of(xt[:], xt_b, D, junk[:], junk_b, st, st_b, 0)
                    kb.op(dve, [xt_b, st_b, gs_b], [tmp_b],
                          lambda: nc.vector.scalar_tensor_tensor(out=tmp[:], in0=xt[:], scalar=rstd, in1=gs[:],
                                                                 op0=ALU.mult, op1=ALU.mult))
                    hb, hb_b = hbs.get()
                    kb.op(pool, [tmp_b, sh_b], [hb_b],
                          lambda: nc.gpsimd.tensor_tensor(out=hb[:], in0=tmp[:], in1=sh[:], op=ALU.add))
                    for k in range(KC):
                        kb.op(pe, [hb_b, ident_b_b], [psT_b],
                              lambda k=k: nc.tensor.transpose(psT[:, k * 128:(k + 1) * 128], hb[:, k * 128:(k + 1) * 128], ident_b[:]))
                    kb.op(act, [psT_b], [hT_b],
                          lambda t=t: nc.scalar.copy(out=hT[:, :, t * 128:(t + 1) * 128],
                                                     in_=psT[:].rearrange("p (k t) -> p k t", k=KC)))
                if is_q:
                    kb.dma(sp, hT_q.rearrange("(k p) t -> p k t", p=128)[:, :, q0:q0 + n], hT[:, :, :n], [hT_b], [kb.dbuf("hT_q")])

                def fm_group(col0, nchunks, rope, dst, dcol0, dkey):
                    stg, stg_b = fm_st.get()
                    for c in range(nchunks):
                        pa, pa_b = psf.get()
                        for k in range(KC):
                            kb.op(pe, [W1_b, hT_b], [pa_b],
                                  lambda k=k: nc.tensor.matmul(pa[:, :n], lhsT=W1[:, k, col0 + c * 128: col0 + (c + 1) * 128],
                                                               rhs=hT[:, k, :n], start=(k == 0), stop=(k == KC - 1)))
                        if rope:
                            pb, pb_b = psf.get()
                            for k in range(KC):
                                kb.op(pe, [W1_b, hT_b], [pb_b],
                                      lambda k=k: nc.tensor.matmul(pb[:, :n], lhsT=W1[:, k, col0 + 512 + c * 128: col0 + 512 + (c + 1) * 128],
                                                                   rhs=hT[:, k, :n], start=(k == 0), stop=(k == KC - 1)))
                            kb.op(dve, [pa_b, cs_b], [r1_b],
                                  lambda: nc.vector.tensor_tensor(out=r1[:, :n], in0=pa[:, :n], in1=cs[:, 0, :n], op=ALU.mult))
                            kb.op(dve, [pb_b, cs_b], [r2_b],
                                  lambda: nc.vector.tensor_tensor(out=r2[:, :n], in0=pb[:, :n], in1=cs[:, 1, :n], op=ALU.mult))
                            kb.op(pool, [r1_b, r2_b], [stg_b],
                                  lambda c=c: nc.gpsimd.tensor_tensor(out=stg[:, c, :n], in0=r1[:, :n], in1=r2[:, :n], op=ALU.add))
                        else:
                            kb.op(act, [pa_b], [stg_b], lambda c=c: nc.scalar.copy(out=stg[:, c, :n], in_=pa[:, :n]))
                    kb.dma(sp, dst.rearrange("(c p) t -> p c t", p=128)[:, :, dcol0:dcol0 + n], stg[:, :nchunks, :n],
                           [stg_b], [kb.dbuf(dkey)])

                if is_q:
                    fm_group(0, 4, True, QA_T, q0, "QA_T")
                    fm_group(2048, 4, False, QB_T, q0, "QB_T")
                fm_group(1024, 4, True, KA_T, e0, "KA_T")
                fm_group(2560, 4, False, KB_T, e0, "KB_T")
                pa, pa_b = psf.get()
                pb, pb_b = psf.get()
                for k in range(KC):
                    kb.op(pe, [W1_b, hT_b], [pa_b],
                          lambda k=k: nc.tensor.matmul(pa[0:32, :n], lhsT=W1[:, k, 3072:3104], rhs=hT[:, k, :n],
                                                       start=(k == 0), stop=(k == KC - 1)))
                for k in range(KC):
                    kb.op(pe, [W1_b, hT_b], [pb_b],
                          lambda k=k: nc.tensor.matmul(pb[0:32, :n], lhsT=W1[:, k, 3104:3136], rhs=hT[:, k, :n],
                                                       start=(k == 0), stop=(k == KC - 1)))
                kb.op(dve, [pa_b, cs_b], [r1_b],
                      lambda: nc.vector.tensor_tensor(out=r1[0:32, :n], in0=pa[0:32, :n], in1=cs[0:32, 2, :n], op=ALU.mult))
                kb.op(dve, [pb_b, cs_b], [r2_b],
                      lambda: nc.vector.tensor_tensor(out=r2[0:32, :n], in0=pb[0:32, :n], in1=cs[0:32, 3, :n], op=ALU.mult))
                kb.op(pool, [r1_b, r2_b], [kpe_b],
                      lambda: nc.gpsimd.tensor_tensor(out=kpe[:, :n], in0=r1[0:32, :n], in1=r2[0:32, :n], op=ALU.add))
                for h in range(C_HEADS):
                    kb.dma(sp, KC_T[h * 96:h * 96 + 32, e0:e0 + n], kpe[:, :n], [kpe_b], [kb.dbuf("KC_T")])

                for t in range(nt):
                    ts_ = slice(t * 128, (t + 1) * 128)
                    for (col0, dst, dkey) in ((FM_COLS, VA, "VA"), (FM_COLS + 512, VB, "VB")):
                        pa, pa_b = psf.get()
                        for k in range(KC):
                            kb.op(pe, [W1_b, hT_b], [pa_b],
                                  lambda k=k: nc.tensor.matmul(pa[:, :], lhsT=hT[:, k, ts_], rhs=W1[:, k, col0:col0 + 512],
                                                               start=(k == 0), stop=(k == KC - 1)))
                        stg, stg_b = tm_st.get()
                        kb.op(act, [pa_b], [stg_b], lambda: nc.scalar.copy(out=stg[:], in_=pa[:, :]))
                        kb.dma(sp, dst[e0 + t * 128:e0 + (t + 1) * 128, :], stg[:], [stg_b], [kb.dbuf(dkey)])
                    for (col0, w, dstT, dstT_b, nk, scol) in ((FM_COLS + 1024, 384, cqnT, cqnT_b, 3, 4), (FM_COLS + 1408, 256, ckvnT, ckvnT_b, 2, 8)):
                        if w == 384 and not is_q:
                            continue
                        pa, pa_b = psf.get()
                        for k in range(KC):
                            kb.op(pe, [W1_b, hT_b], [pa_b],
                                  lambda k=k: nc.tensor.matmul(pa[:, :w], lhsT=hT[:, k, ts_], rhs=W1[:, k, col0:col0 + w],
                                                               start=(k == 0), stop=(k == KC - 1)))
                        rs_ = rstd_of(pa[:, :w], pa_b, w, junk[:, :w], junk_b, st, st_b, scol)
                        cn, cn_b = cqn.get()
                        kb.op(act, [pa_b, st_b], [cn_b],
                              lambda: nc.scalar.activation(out=cn[:, :w], in_=pa[:, :w], func=AF.Copy, scale=rs_))
                        for k in range(nk):
                            kb.op(pe, [cn_b, ident_b_b], [psT_b],
                                  lambda k=k: nc.tensor.transpose(psT[:, k * 128:(k + 1) * 128], cn[:, k * 128:(k + 1) * 128], ident_b[:]))
                        kb.op(act, [psT_b], [dstT_b],
                              lambda: nc.scalar.copy(out=dstT[:, :, ts_], in_=psT[:, 0:nk * 128].rearrange("p (k t) -> p k t", k=nk)))
                    pa, pa_b = psf.get()
                    for k in range(2):
                        kb.op(pe, [Wkv_b, ckvnT_b], [pa_b],
                              lambda k=k: nc.tensor.matmul(pa[:, :], lhsT=ckvnT[:, k, ts_], rhs=Wkv[:, k, 512:1024],
                                                           start=(k == 0), stop=(k == 1)))
                    stg, stg_b = tm_st.get()
                    kb.op(act, [pa_b], [stg_b], lambda: nc.scalar.copy(out=stg[:], in_=pa[:, :]))
                    kb.dma(sp, VC[e0 + t * 128:e0 + (t + 1) * 128, :], stg[:], [stg_b], [kb.dbuf("VC")])
                for h in range(C_HEADS):
                    pa, pa_b = psf.get()
                    for k in range(2):
                        kb.op(pe, [Wkv_b, ckvnT_b], [pa_b],
                              lambda k=k: nc.tensor.matmul(pa[0:64, :n], lhsT=Wkv[:, k, h * 64:(h + 1) * 64], rhs=ckvnT[:, k, :n],
                                                           start=(k == 0), stop=(k == 1)))
                    stg, stg_b = qc_st.get()
                    kb.op(act, [pa_b], [stg_b], lambda: nc.scalar.copy(out=stg[0:64, :n], in_=pa[0:64, :n]))
                    kb.dma(sp, KC_T[h * 96 + 32:h * 96 + 96, e0:e0 + n], stg[0:64, :n], [stg_b], [kb.dbuf("KC_T")])
                if is_q:
                    for h in range(C_HEADS):
                        pa, pa_b = psf.get()
                        pb, pb_b = psf.get()
                        for k in range(3):
                            kb.op(pe, [Wqb_b, cqnT_b], [pa_b],
                                  lambda k=k: nc.tensor.matmul(pa[0:96, :n], lhsT=Wqb[:, k, h * 96:(h + 1) * 96], rhs=cqnT[:, k, :n],
                                                               start=(k == 0), stop=(k == 2)))
                        for k in range(3):
                            kb.op(pe, [Wqb_b, cqnT_b], [pb_b],
                                  lambda k=k: nc.tensor.matmul(pb[0:96, :n], lhsT=Wqb[:, k, 768 + h * 96:768 + (h + 1) * 96], rhs=cqnT[:, k, :n],
                                                               start=(k == 0), stop=(k == 2)))
                        kb.op(dve, [pa_b, cs_b], [r1_b],
                              lambda: nc.vector.tensor_tensor(out=r1[0:96, :n], in0=pa[0:96, :n], in1=cs[0:96, 2, :n], op=ALU.mult))
                        kb.op(dve, [pb_b, cs_b], [r2_b],
                              lambda: nc.vector.tensor_tensor(out=r2[0:96, :n], in0=pb[0:96, :n], in1=cs[0:96, 3, :n], op=ALU.mult))
                        stg, stg_b = qc_st.get()
                        kb.op(pool, [r1_b, r2_b], [stg_b],
                              lambda: nc.gpsimd.tensor_tensor(out=stg[0:96, :n], in0=r1[0:96, :n], in1=r2[0:96, :n], op=ALU.add))
                        kb.dma(sp, QC_T[h * 96:(h + 1) * 96, q0:q0 + n], stg[0:96, :n], [stg_b], [kb.dbuf("QC_T")])

    def phase_attn(kind):
        n_kt = T_EXT // 128
        ctx_kts = list(range(T_ALL // 128, n_kt))
        if kind == "A":
            NH, ROWS, DV, QT_src, KT_src, V_src, Y_dst, scale = A_HEADS, 128, 128, QA_T, KA_T, VA, YA_T, A_HD ** -0.5
            comps = [0, 1]
        else:
            NH, ROWS, DV, QT_src, KT_src, V_src, Y_dst, scale = C_HEADS, 96, 64, QC_T, KC_T, VC, YC_T, 96 ** -0.5
            comps = [0]
        nm = kind
        with ExitStack() as es:
            Vall, Vall_b = _sb(es, nc, "Vall" + nm, [128, n_kt, 512], BF16)
            vsrc = V_src.rearrange("(kt p) e -> p kt e", p=128)
            for k0 in range(0, n_kt, 8):
                k1 = min(n_kt, k0 + 8)
                kb.dma(sp, Vall[:, k0:k1, :], vsrc[:, k0:k1, :], [kb.dbuf("V" + nm)], [Vall_b])
            KTs = Pool8([_sb(es, nc, "KT%s%d" % (nm, i), [ROWS, T_EXT], BF16) for i in range(2)])
            QTs = Pool8([_sb(es, nc, "QT%s%d" % (nm, i), [ROWS, T_Q], BF16) for i in range(2)])
            PTs = Pool8([_sb(es, nc, "PT%s%d" % (nm, i), [128, 512], BF16) for i in range(4)])
            psS = [Pool8([_ps(es, nc, "psS%s%d_%d" % (nm, c, i), [128, 512], F32) for i in range(2 if kind == "A" else 3)]) for c in comps]
            psO = [_ps(es, nc, "psO%s%d" % (nm, c), [128, 512], F32) for c in range(2)]
            psM = [_ps(es, nc, "psM%s%d" % (nm, c), [128, 512], F32) for c in range(2)]
            rs_, rs_b = _sb(es, nc, "ars" + nm, [128, 512], F32)
            oc = [_sb(es, nc, "aoc%s%d" % (nm, c), [128, 512], F32) for c in range(2)]
            osq, osq_b = _sb(es, nc, "aosq" + nm, [128, 512], BF16)
            sd, sd_b = _sb(es, nc, "asd" + nm, [128, 512], F32)
            ysts = Pool8([_sb(es, nc, "ayst%s%d" % (nm, i), [128, 512], BF16) for i in range(2)])
            if kind == "A":
                lamt, lamt_b = _sb(es, nc, "lamt", [128, 4, 64], F32)
                lt, lt_b = _sb(es, nc, "lamtmp", [128, 2, 64], F32)
                ls, ls_b = _sb(es, nc, "lams", [128, 8], F32)
                gsb, gsb_b = _sb(es, nc, "gsubs", [128, 2], F32)
                kb.dma(sp, lamt[:].rearrange("p a d -> p (a d)"), IN("lam").rearrange("a d -> (a d)").partition_broadcast(128), [], [lamt_b])
                kb.dma(sp, gsb[:, 0:1], IN("gsub")[:, :], [], [gsb_b])
                for i in range(2):
                    kb.op(dve, [lamt_b], [lt_b],
                          lambda i=i: nc.vector.tensor_tensor(out=lt[:, i, :], in0=lamt[:, 2 * i, :], in1=lamt[:, 2 * i + 1, :], op=ALU.mult))
                    kb.op(dve, [lt_b], [ls_b],
                          lambda i=i: nc.vector.reduce_sum(out=ls[:, i:i + 1], in_=lt[:, i, :], axis=AX.X))
                kb.op(act, [ls_b], [ls_b], lambda: nc.scalar.activation(out=ls[:, 2:4], in_=ls[:, 0:2], func=AF.Exp))
                kb.op(dve, [ls_b], [ls_b],
                      lambda: nc.vector.tensor_tensor(out=ls[:, 4:5], in0=ls[:, 3:4], in1=ls[:, 2:3], op=ALU.subtract))
                kb.op(dve, [ls_b], [ls_b],
                      lambda: nc.vector.tensor_scalar(out=ls[:, 5:6], in0=ls[:, 4:5], scalar1=-lam_init, scalar2=None, op0=ALU.add))
                kb.op(dve, [gsb_b], [gsb_b],
                      lambda: nc.vector.tensor_scalar(out=gsb[:, 1:2], in0=gsb[:, 0:1], scalar1=1.0 - lam_init, scalar2=None, op0=ALU.mult))
            oi = 0
            for h in range(NH):
                KT, KT_b = KTs.get()
                QT, QT_b = QTs.get()
                kb.dma(sp, KT[:], KT_src[h * ROWS:(h + 1) * ROWS, :], [kb.dbuf("KT" + nm)], [KT_b])
                kb.dma(sp, QT[:], QT_src[h * ROWS:(h + 1) * ROWS, :], [kb.dbuf("QT" + nm)], [QT_b])
                for (q0, n, ext0, is_ctx) in q_chunks(512):
                    kts = ctx_kts if is_ctx else list(range(n_kt))
                    nk = len(kts)
                    if kind == "A":
                        pO = [psO[0], psO[1]]
                        pM = [psM[0], psM[1]]
                    else:
                        pO = [psO[oi % 2]]
                        pM = [psM[oi % 2]]
                        oi += 1
                    S_t = {}

                    def issue_mm1(i):
                        kt = kts[i]
                        for c in comps:
                            ps, ps_b = psS[c].get()
                            r0 = 64 * c if kind == "A" else 0
                            r1 = r0 + 64 if kind == "A" else 96
                            kb.op(pe, [KT_b, QT_b], [ps_b],
                                  lambda: nc.tensor.matmul(ps[:, :n], lhsT=KT[r0:r1, kt * 128:(kt + 1) * 128], rhs=QT[r0:r1, q0:q0 + n],
                                                           start=True, stop=True))
                            S_t[(i, c)] = (ps, ps_b)

                    issue_mm1(0)
                    for i in range(nk):
                        kt = kts[i]
                        if i + 1 < nk:
                            issue_mm1(i + 1)
                        pts = []
                        for c in comps:
                            ps, ps_b = S_t.pop((i, c))
                            PT, PT_b = PTs.get()
                            kb.op(act, [ps_b], [PT_b],
                                  lambda: nc.scalar.activation(out=PT[:, :n], in_=ps[:, :n], func=AF.Exp, scale=scale))
                            pts.append((PT, PT_b))
                        for ci, c in enumerate(comps):
                            PT, PT_b = pts[ci]
                            po, po_b = pO[ci]
                            pm, pm_b = pM[ci]
                            kb.op(pe, [Vall_b, PT_b], [po_b],
                                  lambda: nc.tensor.matmul(po[0:DV, :n], lhsT=Vall[:, kt, h * DV:(h + 1) * DV], rhs=PT[:, :n],
                                                           start=(i == 0), stop=(i == nk - 1)))
                            kb.op(pe, [ones_b_b, PT_b], [pm_b],
                                  lambda: nc.tensor.matmul(pm[0:DV, :n], lhsT=ones_b[:, 0:DV], rhs=PT[:, :n],
                                                           start=(i == 0), stop=(i == nk - 1)))
                    yst, yst_b = ysts.get()
                    if kind == "C":
                        po, po_b = pO[0]
                        pm, pm_b = pM[0]
                        kb.op(dve, [pm_b], [rs_b], lambda: nc.vector.reciprocal(out=rs_[0:DV, :n], in_=pm[0:DV, :n]))
                        kb.op(dve, [po_b, rs_b], [yst_b],
                              lambda: nc.vector.tensor_tensor(out=yst[0:DV, :n], in0=po[0:DV, :n], in1=rs_[0:DV, :n], op=ALU.mult))
                        kb.dma(sp, Y_dst[h * DV:(h + 1) * DV, q0:q0 + n], yst[0:DV, :n], [yst_b], [kb.dbuf("Y" + nm)])
                    else:
                        for c in comps:
                            po, po_b = pO[c]
                            pm, pm_b = pM[c]
                            o_, o_b = oc[c]
                            kb.op(dve, [pm_b], [rs_b], lambda: nc.vector.reciprocal(out=rs_[:, :n], in_=pm[:, :n]))
                            kb.op(dve, [po_b, rs_b], [o_b],
                                  lambda: nc.vector.tensor_tensor(out=o_[:, :n], in0=po[:, :n], in1=rs_[:, :n], op=ALU.mult))
                        o1, o1_b = oc[0]
                        o2, o2_b = oc[1]
                        kb.op(dve, [o1_b, o2_b, ls_b], [o1_b],
                              lambda: nc.vector.scalar_tensor_tensor(out=o1[:, :n], in0=o2[:, :n], scalar=ls[:, 5:6], in1=o1[:, :n],
                                                                     op0=ALU.mult, op1=ALU.add))
                        kb.op(pool, [o1_b], [osq_b],
                              lambda: nc.gpsimd.tensor_tensor(out=osq[:, :n], in0=o1[:, :n], in1=o1[:, :n], op=ALU.mult))
                        pq, pq_b = psS[0].get()
                        kb.op(pe, [ones_b_b, osq_b], [pq_b],
                              lambda: nc.tensor.matmul(pq[:, :n], lhsT=ones_b[:, :], rhs=osq[:, :n], start=True, stop=True))
                        kb.op(act, [pq_b], [sd_b],
                              lambda: nc.scalar.activation(out=sd[:, :n], in_=pq[:, :n], func=AF.Sqrt, scale=1.0 / 128, bias=epsc[:, 0:1]))
                        kb.op(dve, [sd_b], [sd_b], lambda: nc.vector.reciprocal(out=sd[:, :n], in_=sd[:, :n]))
                        kb.op(dve, [o1_b, sd_b, gsb_b], [yst_b],
                              lambda: nc.vector.scalar_tensor_tensor(out=yst[:, :n], in0=o1[:, :n], scalar=gsb[:, 1:2], in1=sd[:, :n],
                                                                     op0=ALU.mult, op1=ALU.mult))
                        kb.dma(sp, Y_dst[h * 128:(h + 1) * 128, q0:q0 + n], yst[:, :n], [yst_b], [kb.dbuf("Y" + nm)])

    def phase_attn_B():
        NT = cfg.ROWS_CORE // 4
        LOC = 256 + T_OWN + 256 + L
        n_lt = LOC // 128
        scale = 64 ** -0.5
        with ExitStack() as es:
            rp, rp_b = _sb(es, nc, "rp", [33, B_HEADS, 15], F32)
            bs, bs_b = _sb(es, nc, "bs", [33, 12, 15], F32)
            bcn, bcn_b = _sb(es, nc, "bcn", [33, 4096], BF16)
            lw = Pool8([_sb(es, nc, "blw%d" % i, [33, 16], BF16) for i in range(3)])
            tst = Pool8([_sb(es, nc, "btst%d" % i, [16, 4096], BF16) for i in range(2)])
            pss = Pool8([_ps(es, nc, "bps%d" % i, [128, 512], F32) for i in range(4)])
            kb.dma(sp, rp[:], IN("rpbT").rearrange("h r i -> r h i"), [], [rp_b])
            kb.dma(sp, bs[:], IN("bsel").rearrange("c b r i -> r (c b) i"), [], [bs_b])
            kb.dma(pool, bcn[:], IN("bconst")[:, :], [], [bcn_b])
            for tt in tst.items:
                kb.op(dve, [], [tt[1]], lambda tt=tt: nc.vector.memset(tt[0][:], 0.0))
            for h in range(B_HEADS):
                for cb in range(12):
                    l_, l_b = lw.get()
                    kb.op(dve, [rp_b, bs_b], [l_b],
                          lambda: nc.vector.tensor_tensor(out=l_[:, 0:15], in0=rp[:, h, :], in1=bs[:, cb, :], op=ALU.mult))
                    st_, st_b2 = tst.get()
                    for sl in range(8):
                        ps, ps_b = pss.get()
                        kb.op(pe, [l_b, bcn_b], [ps_b],
                              lambda: nc.tensor.matmul(ps[0:15, :], lhsT=l_[:, 0:15], rhs=bcn[:, sl * 512:(sl + 1) * 512], start=True, stop=True))
                        kb.op(act, [ps_b], [st_b2], lambda: nc.scalar.copy(out=st_[0:15, sl * 512:(sl + 1) * 512], in_=ps[0:15, :]))
                    kb.dma(sp, btab[h, cb // 4, cb % 4, :, :], st_[:, :], [st_b2], [kb.dbuf("btab")])
        kb.barrier()
        with ExitStack() as es:
            Vall, Vall_b = _sb(es, nc, "VallB", [128, n_lt, 512], BF16)
            vsrc = VB.rearrange("(kt p) e -> p kt e", p=128)
            segs = [(0, T_ALL - 256, 256), (256, 0, T_OWN), (256 + T_OWN, T_OWN, 256), (512 + T_OWN, T_ALL, L)]
            for (l0, e0, ln) in segs:
                for k0 in range(0, ln // 128, 8):
                    k1 = min(ln // 128, k0 + 8)
                    kb.dma(sp, Vall[:, l0 // 128 + k0:l0 // 128 + k1, :], vsrc[:, e0 // 128 + k0:e0 // 128 + k1, :], [kb.dbuf("VB")], [Vall_b])
            KTs = Pool8([_sb(es, nc, "KTB%d" % i, [64, LOC], BF16) for i in range(2)])
            QTs = Pool8([_sb(es, nc, "QTB%d" % i, [64, T_Q], BF16) for i in range(2)])
            BTs = Pool8([_sb(es, nc, "BTB%d" % i, [128, 3, 6, 256], BF16) for i in range(2)])
            PTs = Pool8([_sb(es, nc, "PTB%d" % i, [128, 256], BF16) for i in range(4)])
            psS = Pool8([_ps(es, nc, "psSB%d" % i, [128, 512], F32) for i in range(3)])
            psO = Pool8([_ps(es, nc, "psOB%d" % i, [128, 512], F32) for i in range(2)])
            psM = Pool8([_ps(es, nc, "psMB%d" % i, [128, 512], F32) for i in range(2)])
            rs_, rs_b = _sb(es, nc, "brs", [64, 256], F32)
            ysts = Pool8([_sb(es, nc, "byst%d" % i, [64, 256], BF16) for i in range(2)])
            for h in range(B_HEADS):
                KT, KT_b = KTs.get()
                QT, QT_b = QTs.get()
                BT, BT_b = BTs.get()
                for (l0, e0, ln) in segs:
                    kb.dma(sp, KT[:, l0:l0 + ln], KB_T[h * 64:(h + 1) * 64, e0:e0 + ln], [kb.dbuf("KB_T")], [KT_b])
                kb.dma(sp, QT[:], QB_T[h * 64:(h + 1) * 64, :], [kb.dbuf("QB_T")], [QT_b])
                for cls in range(3):
                    flat = btab[h, cls].rearrange("b i n -> (b i) n")
                    for jt in range(6):
                        for a in range(2):
                            i0 = 11 - (2 * jt + a)
                            src = flat[i0:i0 + 52:17, :].rearrange("b (kc qc) -> kc b qc", qc=64)
                            kb.dma(sp, BT[a * 64:(a + 1) * 64, cls, jt, :].rearrange("p (b qc) -> p b qc", qc=64), src,
                                   [kb.dbuf("btab")], [BT_b])
                work = []
                for t in range(NT):
                    cls = 0 if t == 0 else (2 if t == NT - 1 else 1)
                    tiles = [((256 * t + 128 * jt) // 128, (cls, jt)) for jt in range(6)] + [((512 + T_OWN) // 128 + i, None) for i in range(L // 128)]
                    work.append((t * 256, 256, tiles))
                if not last:
                    work.append((T_OWN, L, [((512 + T_OWN) // 128 + i, None) for i in range(L // 128)]))
                for (q0, n, tiles) in work:
                    po, po_b = psO.get()
                    pm, pm_b = psM.get()
                    nk = len(tiles)
                    for i, (lt_, bias) in enumerate(tiles):
                        ps, ps_b = psS.get()
                        kb.op(pe, [KT_b, QT_b], [ps_b],
                              lambda: nc.tensor.matmul(ps[:, :n], lhsT=KT[:, lt_ * 128:(lt_ + 1) * 128], rhs=QT[:, q0:q0 + n],
                                                       start=True, stop=(bias is None)))
                        if bias is not None:
                            kb.op(pe, [ident_b_b, BT_b], [ps_b],
                                  lambda: nc.tensor.matmul(ps[:, :n], lhsT=ident_b[:, :], rhs=BT[:, bias[0], bias[1], :n],
                                                           start=False, stop=True))
                        PT, PT_b = PTs.get()
                        kb.op(act, [ps_b], [PT_b],
                              lambda: nc.scalar.activation(out=PT[:, :n], in_=ps[:, :n], func=AF.Exp, scale=scale))
                        kb.op(pe, [Vall_b, PT_b], [po_b],
                              lambda: nc.tensor.matmul(po[0:64, :n], lhsT=Vall[:, lt_, h * 64:(h + 1) * 64], rhs=PT[:, :n],
                                                       start=(i == 0), stop=(i == nk - 1)))
                        kb.op(pe, [ones_b_b, PT_b], [pm_b],
                              lambda: nc.tensor.matmul(pm[0:64, :n], lhsT=ones_b[:, 0:64], rhs=PT[:, :n],
                                                       start=(i == 0), stop=(i == nk - 1)))
                    yst, yst_b = ysts.get()
                    kb.op(dve, [pm_b], [rs_b], lambda: nc.vector.reciprocal(out=rs_[:, :n], in_=pm[0:64, :n]))
                    kb.op(dve, [po_b, rs_b], [yst_b],
                          lambda: nc.vector.tensor_tensor(out=yst[:, :n], in0=po[0:64, :n], in1=rs_[:, :n], op=ALU.mult))
                    kb.dma(sp, YB_T[h * 64:(h + 1) * 64, q0:q0 + n], yst[:, :n], [yst_b], [kb.dbuf("YB_T")])

    def phase_merge():
        with ExitStack() as es:
            Wg, Wg_b = _sb(es, nc, "Wg", [128, KC, 3 * D], BF16)
            for k in range(KC):
                for c0 in range(0, 3 * D, 1536):
                    kb.dma(pool, Wg[:, k, c0:c0 + 1536], IN("wg")[k * 128:(k + 1) * 128, c0:c0 + 1536], [], [Wg_b])
            Wbr, Wbr_b = _sb(es, nc, "Wbr", [128, 3, 4, D], BF16)
            for i in range(3):
                for k in range(4):
                    kb.dma(pool, Wbr[:, i, k, :], IN("wbr")[i, k * 128:(k + 1) * 128, :], [], [Wbr_b])
            Wo, Wo_b = _sb(es, nc, "Wo", [128, KC, D], BF16)
            for k in range(KC):
                kb.dma(pool, Wo[:, k, :], IN("wout")[k * 128:(k + 1) * 128, :], [], [Wo_b])
            Wrf, Wrf_b = _sb(es, nc, "Wrf", [128, KC, 32], F32)
            Wr, Wr_b = _sb(es, nc, "Wr", [128, 2, KC, 32], BF16)
            kb.dma(sp, Wrf[:], IN("wr").rearrange("(k p) e -> p k e", p=128), [], [Wrf_b])
            kb.op(dve, [Wrf_b], [Wr_b], lambda: nc.vector.tensor_copy(out=Wr[:, 0, :, :], in_=Wrf[:]))
            kb.op(dve, [Wrf_b, Wr_b], [Wr_b],
                  lambda: nc.vector.tensor_tensor(out=Wr[:, 1, :, :], in0=Wrf[:], in1=Wr[:, 0, :, :], op=ALU.subtract))
            brb, brb_b = _sb(es, nc, "brb", [128, 32], F32)
            kb.dma(sp, brb[:], IN("brt").partition_broadcast(128), [], [brb_b])
            bc = {}
            for who, a in (("l", 0), ("c", 1)):
                if who == "c" and last:
                    continue
                for nm2, si in (("gt1", 2), ("sh2", 3), ("gs2", 4)):
                    bc[(who, nm2)] = load_bc(es, "bc_%s_%s" % (who, nm2), modv[a, si:si + 1, :])
            Ys = [_sb(es, nc, "Yin%d" % i, [128, 4, 512], BF16) for i in range(3)]
            hT, hT_b = _sb(es, nc, "mhT", [128, KC, 512], BF16)
            mT, mT_b = _sb(es, nc, "mT", [128, KC, 512], BF16)
            sigs = Pool8([_sb(es, nc, "msig%d" % i, [128, 512], F32) for i in range(2)])
            tmps = Pool8([_sb(es, nc, "mtmp%d" % i, [128, 512], F32) for i in range(2)])
            macc, macc_b = _sb(es, nc, "macc", [128, 512], F32)
            xts = Pool8([_sb(es, nc, "mxt%d" % i, [128, D], F32) for i in range(2)])
            xns = Pool8([_sb(es, nc, "mxn%d" % i, [128, D], F32) for i in range(2)])
            t2, t2_b = _sb(es, nc, "mt2", [128, D], F32)
            h2, h2_b = _sb(es, nc, "mh2", [128, D], F32)
            junk, junk_b = _sb(es, nc, "mjunk", [128, D], BF16)
            h2Tf, h2Tf_b = _sb(es, nc, "h2Tlo", [128, KC, 128], BF16)
            hbs = Pool8([_sb(es, nc, "mhb%d" % i, [128, 2, D], BF16) for i in range(2)])
            h2Tb, h2Tb_b = _sb(es, nc, "h2Tb", [128, KC, 512], BF16)
            st, st_b = _sb(es, nc, "mst", [128, 8], F32)
            rt, rt_b = _sb(es, nc, "mrt", [128, 4, 32], F32)
            top, top_b = _sb(es, nc, "mtop", [128, 16], F32)
            gws = Pool8([_sb(es, nc, "mgw%d" % i, [128, 32], F32) for i in range(2)])
            psf = Pool8([_ps(es, nc, "mps%d" % i, [128, 512], F32) for i in range(6)])
            psTa = _ps(es, nc, "mpsTa", [128, 1024], BF16)
            psTb = _ps(es, nc, "mpsTb", [128, 1024], BF16)
            YT = (YA_T, YB_T, YC_T)
            CUT = 99
            for (q0, n, ext0, is_ctx) in q_chunks(512):
                if CUT <= 0:
                    break
                who = "c" if is_ctx else "l"
                nt = n // 128
                for i in range(3):
                    kb.dma(sp, Ys[i][0][:, :, :n], YT[i].rearrange("(k p) t -> p k t", p=128)[:, :, q0:q0 + n], [kb.dbuf("Y%d" % i)], [Ys[i][1]])
                kb.dma(sp, hT[:, :, :n], hT_q.rearrange("(k p) t -> p k t", p=128)[:, :, q0:q0 + n], [kb.dbuf("hT_q")], [hT_b])
                for fc in range(KC):
                    for i in range(3):
                        pz, pz_b = psf.get()
                        pg, pg_b = psf.get()
                        for k in range(4):
                            kb.op(pe, [Wbr_b, Ys[i][1]], [pz_b],
                                  lambda: nc.tensor.matmul(pz[:, :n], lhsT=Wbr[:, i, k, fc * 128:(fc + 1) * 128], rhs=Ys[i][0][:, k, :n],
                                                           start=(k == 0), stop=(k == 3)))
                        for k in range(KC):
                            kb.op(pe, [Wg_b, hT_b], [pg_b],
                                  lambda: nc.tensor.matmul(pg[:, :n], lhsT=Wg[:, k, i * D + fc * 128:i * D + (fc + 1) * 128], rhs=hT[:, k, :n],
                                                           start=(k == 0), stop=(k == KC - 1)))
                        sg, sg_b = sigs.get()
                        kb.op(act, [pg_b], [sg_b], lambda: nc.scalar.activation(out=sg[:, :n], in_=pg[:, :n], func=AF.Sigmoid))
                        if i == 0:
                            kb.op(dve, [pz_b, sg_b], [macc_b],
                                  lambda: nc.vector.tensor_tensor(out=macc[:, :n], in0=pz[:, :n], in1=sg[:, :n], op=ALU.mult))
                        else:
                            tp, tp_b = tmps.get()
                            kb.op(dve, [pz_b, sg_b], [tp_b],
                                  lambda: nc.vector.tensor_tensor(out=tp[:, :n], in0=pz[:, :n], in1=sg[:, :n], op=ALU.mult))
                            if i == 1:
                                kb.op(pool, [tp_b, macc_b], [macc_b],
                                      lambda: nc.gpsimd.tensor_tensor(out=macc[:, :n], in0=macc[:, :n], in1=tp[:, :n], op=ALU.add))
                            else:
                                kb.op(pool, [tp_b, macc_b], [mT_b],
                                      lambda: nc.gpsimd.tensor_tensor(out=mT[:, fc, :n], in0=macc[:, :n], in1=tp[:, :n], op=ALU.add))
                if CUT <= 1:
                    continue
                gt1, gt1_b = bc[(who, "gt1")]
                gs2, gs2_b = bc[(who, "gs2")]
                sh2, sh2_b = bc[(who, "sh2")]
                for t in range(nt):
                    ts_ = slice(t * 128, (t + 1) * 128)
                    xt, xt_b = xts.get()
                    src = IN("ctx")[ext0 - T_ALL + t * 128: ext0 - T_ALL + (t + 1) * 128, :] if is_ctx else IN("x_ext")[ext0 + t * 128:ext0 + (t + 1) * 128, :]
                    kb.dma(sp, xt[:], src, [], [xt_b])
                    for nn in range(2):
                        py, py_b = psf.get()
                        for fc in range(KC):
                            kb.op(pe, [mT_b, Wo_b], [py_b],
                                  lambda: nc.tensor.matmul(py[:, :], lhsT=mT[:, fc, ts_], rhs=Wo[:, fc, nn * 512:(nn + 1) * 512],
                                                           start=(fc == 0), stop=(fc == KC - 1)))
                        kb.op(dve, [py_b, gt1_b], [t2_b],
                              lambda: nc.vector.tensor_tensor(out=t2[:, nn * 512:(nn + 1) * 512], in0=py[:, :], in1=gt1[:, nn * 512:(nn + 1) * 512], op=ALU.mult))
                    xn, xn_b = xns.get()
                    kb.op(pool, [t2_b, xt_b], [xn_b], lambda: nc.gpsimd.tensor_tensor(out=xn[:], in0=t2[:], in1=xt[:], op=ALU.add))
                    kb.dma(sp, x_res[q0 + t * 128:q0 + (t + 1) * 128, :], xn[:], [xn_b], [kb.dbuf("x_res")])
                    if CUT <= 2:
                        continue
                    rstd = rstd_of(xn[:], xn_b, D, junk[:], junk_b, st, st_b, 0)
                    kb.op(dve, [xn_b, st_b, gs2_b], [t2_b],
                          lambda: nc.vector.scalar_tensor_tensor(out=t2[:], in0=xn[:], scalar=rstd, in1=gs2[:], op0=ALU.mult, op1=ALU.mult))
                    kb.op(pool, [t2_b, sh2_b], [h2_b], lambda: nc.gpsimd.tensor_tensor(out=h2[:], in0=t2[:], in1=sh2[:], op=ALU.add))
                    hb, hb_b = hbs.get()
                    kb.op(act, [h2_b], [hb_b], lambda: nc.scalar.copy(out=hb[:, 0, :], in_=h2[:]))
                    kb.op(dve, [h2_b, hb_b], [hb_b],
                          lambda: nc.vector.tensor_tensor(out=hb[:, 1, :], in0=h2[:], in1=hb[:, 0, :], op=ALU.subtract))
                    for part, (pt_, pt_b) in enumerate((psTa, psTb)):
                        for k in range(KC):
                            kb.op(pe, [hb_b, ident_b_b], [pt_b],
                                  lambda: nc.tensor.transpose(pt_[:, k * 128:(k + 1) * 128], hb[:, part, k * 128:(k + 1) * 128], ident_b[:]))
                    kb.op(act, [psTa[1]], [h2Tb_b],
                          lambda: nc.scalar.copy(out=h2Tb[:, :, ts_], in_=psTa[0][:].rearrange("p (k t) -> p k t", k=KC)))
                    kb.op(dve, [psTb[1]], [h2Tf_b],
                          lambda: nc.vector.tensor_copy(out=h2Tf[:], in_=psTb[0][:].rearrange("p (k t) -> p k t", k=KC)))
                    if CUT <= 3:
                        continue
                    pl, pl_b = psf.get()
                    for k in range(KC):
                        kb.op(pe, [h2Tb_b, Wr_b], [pl_b],
                              lambda: nc.tensor.matmul(pl[:, 0:32], lhsT=h2Tb[:, k, ts_], rhs=Wr[:, 0, k, :], start=(k == 0), stop=False))
                        kb.op(pe, [h2Tb_b, Wr_b], [pl_b],
                              lambda: nc.tensor.matmul(pl[:, 0:32], lhsT=h2Tb[:, k, ts_], rhs=Wr[:, 1, k, :], start=False, stop=False))
                        kb.op(pe, [h2Tf_b, Wr_b], [pl_b],
                              lambda: nc.tensor.matmul(pl[:, 0:32], lhsT=h2Tf[:, k, :], rhs=Wr[:, 0, k, :], start=False, stop=(k == KC - 1)))
                    kb.op(dve, [pl_b, brb_b], [rt_b], lambda: nc.vector.tensor_tensor(out=rt[:, 0, :], in0=pl[:, 0:32], in1=brb[:], op=ALU.add))
                    kb.op(dve, [rt_b], [top_b], lambda: nc.vector.max(out=top[:, 0:8], in_=rt[:, 0, :]))
                    kb.op(dve, [rt_b, top_b], [rt_b],
                          lambda: nc.vector.tensor_scalar(out=rt[:, 1, :], in0=rt[:, 0, :], scalar1=top[:, 3:4], scalar2=None, op0=ALU.is_ge))
                    kb.op(dve, [top_b], [top_b],
                          lambda: nc.vector.tensor_scalar(out=top[:, 8:9], in0=top[:, 0:1], scalar1=-1.0, scalar2=None, op0=ALU.mult))
                    kb.op(act, [rt_b, top_b], [rt_b],
                          lambda: nc.scalar.activation(out=rt[:, 2, :], in_=rt[:, 0, :], func=AF.Exp, bias=top[:, 8:9], scale=1.0))
                    kb.op(dve, [rt_b], [rt_b], lambda: nc.vector.tensor_tensor(out=rt[:, 3, :], in0=rt[:, 2, :], in1=rt[:, 1, :], op=ALU.mult))
                    kb.op(dve, [rt_b], [top_b], lambda: nc.vector.reduce_sum(out=top[:, 9:10], in_=rt[:, 3, :], axis=AX.X))
                    kb.op(dve, [top_b], [top_b], lambda: nc.vector.reciprocal(out=top[:, 10:11], in_=top[:, 9:10]))
                    gw, gw_b = gws.get()
                    kb.op(dve, [rt_b, top_b], [gw_b],
                          lambda: nc.vector.tensor_scalar(out=gw[:], in0=rt[:, 3, :], scalar1=top[:, 10:11], scalar2=None, op0=ALU.mult))
                    kb.dma(sp, gw_d[q0 + t * 128:q0 + (t + 1) * 128, :], gw[:], [gw_b], [kb.dbuf("gw_d")])
                kb.dma(sp, h2T.rearrange("(k p) t -> p k t", p=128)[:, :, q0:q0 + n], h2Tb[:, :, :n], [h2Tb_b], [kb.dbuf("h2T")])

    def phase_moe():
        with ExitStack() as es:
            gus = Pool8([_sb(es, nc, "gu%d" % i, [128, KC, 2 * D], BF16) for i in range(2)])
            dns = Pool8([_sb(es, nc, "dn%d" % i, [128, KC, D], BF16) for i in range(2)])
            hT, hT_b = _sb(es, nc, "eh2T", [128, KC, 512], BF16)
            acc, acc_b = _sb(es, nc, "eacc", [128, 4, D], F32)
            aTs = Pool8([_sb(es, nc, "eaT%d" % i, [128, KC, 512], BF16) for i in range(2)])
            ggs = Pool8([_sb(es, nc, "egg%d" % i, [128, 512], F32) for i in range(3)])
            sss = Pool8([_sb(es, nc, "ess%d" % i, [128, 512], F32) for i in range(3)])
            uus = Pool8([_sb(es, nc, "euu%d" % i, [128, 512], F32) for i in range(3)])
            gw, gw_b = _sb(es, nc, "egw", [128, 4, 32], F32)
            sel, sel_b = _sb(es, nc, "esel", [32, NE, 128], BF16)
            bdf, bdf_b = _sb(es, nc, "ebdf", [32, D], F32)
            bd, bd_b = _sb(es, nc, "ebd", [32, D], BF16)
            bg, bg_b = _sb(es, nc, "ebg", [128, NE, 16], F32)
            xts = Pool8([_sb(es, nc, "ext%d" % i, [128, D], F32) for i in range(2)])
            xns = Pool8([_sb(es, nc, "exn%d" % i, [128, D], F32) for i in range(2)])
            t2, t2_b = _sb(es, nc, "et2", [128, D], F32)
            junk, junk_b = _sb(es, nc, "ejunk", [128, D], BF16)
            st, st_b = _sb(es, nc, "est", [128, 8], F32)
            psf = Pool8([_ps(es, nc, "eps%d" % i, [128, 512], F32) for i in range(8)])
            kb.op(dve, [], [bdf_b], lambda: nc.vector.memset(bdf[:], 0.0))
            kb.dma(sp, bdf[0:NE, :], IN("bdn")[:, :], [], [bdf_b])
            kb.op(dve, [bdf_b], [bd_b], lambda: nc.vector.tensor_copy(out=bd[:], in_=bdf[:]))
            for e in range(NE):
                kb.op(dve, [ones_f_b, ident_f_b], [sel_b],
                      lambda: nc.vector.tensor_scalar(out=sel[:, e, :], in0=ones_f[0:32, :], scalar1=ident_f[0:32, e:e + 1], scalar2=None, op0=ALU.mult))
            kb.dma(sp, bg[:], IN("bgu")[:, :, :], [], [bg_b])
            gt2l, gt2l_b = load_bc(es, "gt2l", modv[0, 5:6, :])
            if not last:
                gt2c, gt2c_b = load_bc(es, "gt2c", modv[1, 5:6, :])
            else:
                gfin, gfin_b = _sb(es, nc, "gfin", [128, D], F32)
                kb.dma(sp, gfin[:], IN("gvecs")[2:3, :].partition_broadcast(128), [], [gfin_b])
            def load_expert(e_):
                gu, gu_b = gus.get()
                dn, dn_b = dns.get()
                for k in range(KC):
                    kb.dma(pool, gu[:, k, :], IN("wgu")[e_, k * 128:(k + 1) * 128, :], [], [gu_b])
                for k in range(KC):
                    kb.dma(pool, dn[:, k, :], IN("wdn")[e_, k * 128:(k + 1) * 128, :], [], [dn_b])
                return (gu, gu_b, dn, dn_b)

            chunks_ = q_chunks(512)
            wq = [load_expert(0)]
            for ci, (q0, n, ext0, is_ctx) in enumerate(chunks_):
                nt = n // 128
                kb.dma(sp, hT[:, :, :n], h2T.rearrange("(k p) t -> p k t", p=128)[:, :, q0:q0 + n], [kb.dbuf("h2T")], [hT_b])
                kb.dma(sp, gw[:, :nt, :], gw_d[q0:q0 + n, :].rearrange("(t p) e -> p t e", p=128), [kb.dbuf("gw_d")], [gw_b])
                kb.op(dve, [], [acc_b], lambda: nc.vector.memset(acc[:], 0.0))
                for e in range(NE):
                    gu, gu_b, dn, dn_b = wq.pop(0)
                    nxt = e + 1 if e + 1 < NE else (0 if ci + 1 < len(chunks_) else None)
                    if nxt is not None:
                        wq.append(load_expert(nxt))
                    aT, aT_b = aTs.get()
                    pend = None

                    def finish(p):
                        fc_, (g_, g_b), (s2, s2_b), (u_, u_b) = p
                        kb.op(dve, [g_b, s2_b], [g_b], lambda: nc.vector.tensor_tensor(out=g_[:, :n], in0=g_[:, :n], in1=s2[:, :n], op=ALU.mult))
                        kb.op(dve, [u_b, g_b], [aT_b], lambda: nc.vector.tensor_tensor(out=aT[:, fc_, :n], in0=u_[:, :n], in1=g_[:, :n], op=ALU.mult))

                    for fc in range(KC):
                        pg, pg_b = psf.get()
                        pu, pu_b = psf.get()
                        for k in range(KC):
                            kb.op(pe, [gu_b, hT_b], [pg_b],
                                  lambda: nc.tensor.matmul(pg[:, :n], lhsT=gu[:, k, fc * 128:(fc + 1) * 128], rhs=hT[:, k, :n], start=(k == 0), stop=(k == KC - 1)))
                        for k in range(KC):
                            kb.op(pe, [gu_b, hT_b], [pu_b],
                                  lambda: nc.tensor.matmul(pu[:, :n], lhsT=gu[:, k, D + fc * 128:D + (fc + 1) * 128], rhs=hT[:, k, :n], start=(k == 0), stop=(k == KC - 1)))
                        g_, g_b = ggs.get()
                        s2, s2_b = sss.get()
                        u_, u_b = uus.get()
                        kb.op(dve, [pg_b, bg_b], [g_b],
                              lambda: nc.vector.tensor_scalar(out=g_[:, :n], in0=pg[:, :n], scalar1=bg[:, e, fc:fc + 1], scalar2=LIM, op0=ALU.add, op1=ALU.min))
                        kb.op(act, [g_b], [s2_b], lambda: nc.scalar.activation(out=s2[:, :n], in_=g_[:, :n], func=AF.Sigmoid, scale=ALPHA))
                        kb.op(dve, [pu_b, bg_b], [u_b],
                              lambda: nc.vector.tensor_scalar(out=u_[:, :n], in0=pu[:, :n], scalar1=bg[:, e, 8 + fc:9 + fc], scalar2=LIM, op0=ALU.add, op1=ALU.min))
                        kb.op(dve, [u_b], [u_b],
                              lambda: nc.vector.tensor_scalar(out=u_[:, :n], in0=u_[:, :n], scalar1=-LIM, scalar2=1.0, op0=ALU.max, op1=ALU.add))
                        if pend is not None:
                            finish(pend)
                        pend = (fc, (g_, g_b), (s2, s2_b), (u_, u_b))
                    finish(pend)
                    for t in range(nt):
                        for nn in range(2):
                            py, py_b = psf.get()
                            kb.op(pe, [sel_b, bd_b], [py_b],
                                  lambda: nc.tensor.matmul(py[:, :], lhsT=sel[:, e, :], rhs=bd[:, nn * 512:(nn + 1) * 512], start=True, stop=False))
                            for fc in range(KC):
                                kb.op(pe, [aT_b, dn_b], [py_b],
                                      lambda: nc.tensor.matmul(py[:, :], lhsT=aT[:, fc, t * 128:(t + 1) * 128], rhs=dn[:, fc, nn * 512:(nn + 1) * 512],
                                                               start=False, stop=(fc == KC - 1)))
                            kb.op(dve, [py_b, gw_b, acc_b], [acc_b],
                                  lambda: nc.vector.scalar_tensor_tensor(out=acc[:, t, nn * 512:(nn + 1) * 512], in0=py[:, :], scalar=gw[:, t, e:e + 1],
                                                                         in1=acc[:, t, nn * 512:(nn + 1) * 512], op0=ALU.mult, op1=ALU.add))
                gt2, gt2_b = (gt2c, gt2c_b) if is_ctx else (gt2l, gt2l_b)
                for t in range(nt):
                    xt, xt_b = xts.get()
                    kb.dma(sp, xt[:], x_res[q0 + t * 128:q0 + (t + 1) * 128, :], [kb.dbuf("x_res")], [xt_b])
                    kb.op(dve, [acc_b, gt2_b], [t2_b], lambda: nc.vector.tensor_tensor(out=t2[:], in0=acc[:, t, :], in1=gt2[:], op=ALU.mult))
                    xn, xn_b = xns.get()
                    kb.op(pool, [t2_b, xt_b], [xn_b], lambda: nc.gpsimd.tensor_tensor(out=xn[:], in0=t2[:], in1=xt[:], op=ALU.add))
                    if last:
                        rstd = rstd_of(xn[:], xn_b, D, junk[:], junk_b, st, st_b, 0)
                        kb.op(dve, [xn_b, st_b, gfin_b], [t2_b],
                              lambda: nc.vector.scalar_tensor_tensor(out=t2[:], in0=xn[:], scalar=rstd, in1=gfin[:], op0=ALU.mult, op1=ALU.mult))
                        kb.dma(sp, out_x[q0 + t * 128:q0 + (t + 1) * 128, :], t2[:], [t2_b], [], is_output=True)
                    else:
                        kb.dma(sp, out_x[q0 + t * 128:q0 + (t + 1) * 128, :], xn[:], [xn_b], [], is_output=True)

    phases = [phase0, phase1, lambda: phase_attn("A"), lambda: phase_attn("C"), phase_attn_B, phase_merge, phase_moe]
    for i, ph in enumerate(phases):
        if i <= upto:
            ph()
            kb.barrier()
    kb.finish()
    es0.close()
    return nc


def _partner(nf):
    d = np.arange(4 * nf)
    return np.where((d // nf) % 2 == 0, d + nf, d - nf)


def _rope_full(pos, rot_dim):
    nf = rot_dim // 4
    rows = (pos // GRID_W).astype(np.float32)
    cols = (pos % GRID_W).astype(np.float32)
    inv = (np.float32(10000.0) ** (-np.arange(nf, dtype=np.float32) / np.float32(nf))).astype(np.float32)
    ar = (rows[:, None] * inv).astype(np.float32)
    ac = (cols[:, None] * inv).astype(np.float32)
    cos = np.concatenate([np.cos(ar), np.cos(ar), np.cos(ac), np.cos(ac)], axis=1).astype(np.float32)
    sin = np.concatenate([-np.sin(ar), np.sin(ar), -np.sin(ac), np.sin(ac)], axis=1).astype(np.float32)
    return cos.T.copy(), sin.T.copy()


def host_tables(cfg, j):
    T_OWN, T_ALL, L, T_EXT = cfg.T_OWN, cfg.T_ALL, cfg.L, cfg.T_EXT
    pos = np.concatenate([np.arange(T_OWN) + j * T_OWN, np.arange(T_OWN) + (1 - j) * T_OWN]).astype(np.int64)
    ca, sa = _rope_full(pos, 64)
    cc, sc = _rope_full(pos, 32)
    cosA = np.ones((128, T_EXT), np.float32)
    sinA = np.zeros((128, T_EXT), np.float32)
    cosA[0:64, :T_ALL] = ca
    cosA[64:128, :T_ALL] = ca
    sinA[0:64, :T_ALL] = sa
    sinA[64:128, :T_ALL] = sa
    cosC = np.ones((96, T_EXT), np.float32)
    sinC = np.zeros((96, T_EXT), np.float32)
    cosC[0:32, :T_ALL] = cc
    sinC[0:32, :T_ALL] = sc
    return {"cosA": cosA, "sinA": sinA, "cosC": cosC, "sinC": sinC}


def host_bias_consts(cfg, j):
    c = np.arange(GRID_W)
    col_start = np.clip(c - 8, 0, GRID_W - 16)
    colmask = (c[None, :] >= col_start[:, None]) & (c[None, :] < col_start[:, None] + 16)
    bc = np.zeros((33, GRID_W, GRID_W), np.float32)
    for r in range(31):
        kc_, qc_ = np.meshgrid(c, c, indexing="ij")
        bc[r] = 8.0 * ((kc_ - qc_ + 15) == r) * colmask.T
    bc[31] = NEG * (1.0 - colmask.T)
    bc[32] = NEG * colmask.T
    def valid(kind, b, dr):
        if kind == "interior":
            return -4 <= dr <= 3
        if kind == "first":
            return 0 <= b + dr <= 7
        if kind == "last":
            return -4 <= b + dr <= 3
    kinds = [("first" if j == 0 else "interior"), "interior", ("last" if j == 1 else "interior")]
    sel = np.zeros((3, 4, 33, 15), np.float32)
    for ci, kind in enumerate(kinds):
        for b in range(4):
            for ip in range(15):
                dr = 7 - ip
                if valid(kind, b, dr):
                    sel[ci, b, 0:32, ip] = 1.0
                else:
                    sel[ci, b, 31, ip] = 1.0
                    sel[ci, b, 32, ip] = 1.0
    return {"bconst": bc.reshape(33, 4096), "bsel": sel}


def host_layer(cfg, inp, l):
    f = lambda a: np.ascontiguousarray(a, dtype=np.float32)
    w_in = inp["w_in"][l]
    qa, ka, va = w_in[:, 0:512], w_in[:, 512:1024], w_in[:, 1024:1536]
    qb, kb_, vb = w_in[:, 1536:2048], w_in[:, 2048:2560], w_in[:, 2560:3072]
    cq, ckv, ckr = w_in[:, 3072:3456], w_in[:, 3456:3712], w_in[:, 3712:3744]
    gates = w_in[:, 3744:6816]
    pA = _partner(16)
    idxA = (np.arange(512) // 64) * 64
    swA = idxA + pA[np.arange(512) % 64]
    pC = _partner(8)
    w1 = np.concatenate([qa, qa[:, swA], ka, ka[:, swA], qb, kb_, ckr, ckr[:, pC], va, vb, cq, ckv], axis=1)
    wq = inp["w_q_b"][l]
    main = np.zeros((C_QR, 768), np.float32)
    sw = np.zeros((C_QR, 768), np.float32)
    for h in range(C_HEADS):
        pe_ = wq[:, h * 96 + 64:h * 96 + 96]
        main[:, h * 96:h * 96 + 32] = pe_
        main[:, h * 96 + 32:h * 96 + 96] = wq[:, h * 96:h * 96 + 64]
        sw[:, h * 96:h * 96 + 32] = pe_[:, pC]
    wkv = inp["w_kv_b"][l].reshape(C_KVR, C_HEADS, 128)
    wkvb = np.concatenate([wkv[:, :, :64].reshape(C_KVR, 512), wkv[:, :, 64:].reshape(C_KVR, 512)], axis=1)
    rpb = inp["rpb"][l]
    rpbT = np.zeros((B_HEADS, 33, 15), np.float32)
    rpbT[:, 0:31, :] = np.transpose(rpb[:, ::-1, :], (0, 2, 1))
    rpbT[:, 31, :] = 1.0
    rpbT[:, 32, :] = 1.0
    wgu = inp["w_gate_up"][l]
    NE = cfg.NE
    wgu2 = np.concatenate([wgu[:NE, :, 0::2], wgu[:NE, :, 1::2]], axis=2)
    bgu = inp["b_gate_up"][l][:NE]
    bg = bgu[:, 0::2].reshape(NE, 8, 128)
    bu = bgu[:, 1::2].reshape(NE, 8, 128)
    bgu2 = np.concatenate([np.transpose(bg, (2, 0, 1)), np.transpose(bu, (2, 0, 1))], axis=2)
    d = {
        "w_mod": f(inp["w_mod"][l]), "b_mod": f(inp["b_mod"][l][None, :]),
        "gvecs": f(np.stack([inp["g_mix"][l], inp["g_ffn"][l], inp["g_final"]])),
        "w1": f(w1), "wg": f(gates), "ident": np.eye(128, dtype=np.float32),
        "lam": f(np.stack([inp["lam_q1"][l], inp["lam_k1"][l], inp["lam_q2"][l], inp["lam_k2"][l]])),
        "gsub": f(inp["g_subln"][l].reshape(128, 1)),
        "gqa": f(inp["g_q_a"][l].reshape(3, 128).T), "gkva": f(inp["g_kv_a"][l].reshape(2, 128).T),
        "wqb": f(np.concatenate([main, sw], axis=1)), "wkvb": f(wkvb), "rpbT": f(rpbT),
        "wbr": f(np.stack([inp["w_br_a"][l], inp["w_br_b"][l], inp["w_br_c"][l]])),
        "wout": f(inp["w_out"][l]), "wr": f(inp["w_router"][l][:, :NE] if NE == 32 else np.pad(inp["w_router"][l][:, :NE], ((0, 0), (0, 32 - NE)))),
        "brt": f((inp["b_router"][l][:NE] if NE == 32 else np.pad(inp["b_router"][l][:NE], (0, 32 - NE), constant_values=-1e4))[None, :]),
        "wgu": f(wgu2), "bgu": f(bgu2), "wdn": f(inp["w_down"][l][:NE]), "bdn": f(inp["b_down"][l][:NE]),
    }
    return d


def host_core(cfg, x_batch, ctx_b, c_b, c_ctx, j):
    T_OWN = cfg.T_OWN
    own = x_batch[j * T_OWN:(j + 1) * T_OWN]
    oth = x_batch[(1 - j) * T_OWN:(2 - j) * T_OWN]
    cvec = np.stack([c_b.reshape(KC, 128).T, c_ctx.reshape(KC, 128).T], axis=2)
    d = {"x_ext": np.ascontiguousarray(np.concatenate([own, oth], axis=0), dtype=np.float32),
         "ctx": np.ascontiguousarray(ctx_b, dtype=np.float32),
         "cvec": np.ascontiguousarray(cvec, dtype=np.float32)}
    d.update(host_tables(cfg, j))
    d.update(host_bias_consts(cfg, j))
    return d


N_CORES = 8


def _run_layer(cfg, inp, layer, last, x_full, ctx_full):
    nc = build_program(cfg, layer, last)
    lay = host_layer(cfg, inp, layer)
    maps = []
    for core in range(N_CORES):
        b, j = core // 2, core % 2
        d = dict(lay)
        d.update(host_core(cfg, x_full[b], ctx_full[b], inp["c"][b], inp["c_ctx"], j))
        maps.append(d)
    res = run_bass_kernel_spmd(nc, maps, core_ids=list(range(N_CORES)))
    return [np.asarray(r["out_x"]) for r in res.results]


def kernel(**inputs):
    inp = {k: np.asarray(v) for k, v in inputs.items()}
    B, SEQ, _ = inp["x"].shape
    L = inp["ctx"].shape[1]
    cfg = Cfg(t_own=SEQ // 2, ctx=L, n_exp=inp["w_router"].shape[2], depth=inp["w_mod"].shape[0])
    T_OWN = cfg.T_OWN
    x = inp["x"].astype(np.float32)
    xc = inp["ctx"].astype(np.float32)
    depth = cfg.DEPTH
    for layer in range(depth):
        last = layer == depth - 1
        outs = _run_layer(cfg, inp, layer, last, x, xc)
        x_new = np.empty_like(x)
        for core in range(N_CORES):
            b, j = core // 2, core % 2
            x_new[b, j * T_OWN:(j + 1) * T_OWN] = outs[core][:T_OWN]
        if not last:
            xc = np.stack([outs[2 * b][T_OWN:] for b in range(B)], axis=0)
        x = x_new
    return x
```
